# Optimizing a Trainium2 kernel written in Bass

```python
import math
import jax, jax.numpy as jnp
from jax import lax
import numpy as np

D_MODEL = 2048
BATCH = 2
SEQ = 4096
DEPTH = 2
DEC_BATCH = 16
DEC_SEQ = 64
PAST_LEN = 2048

CHUNK = 64
N_META = 16
BRANCH_WIDTH = D_MODEL // 2
A_HEADS = 8
A_HEAD_DIM = BRANCH_WIDTH // A_HEADS
IDX_HEADS = 16
IDX_DIM = 64
TOPK_MAX = 256
QBLK = 64
T5_BUCKETS = 32
T5_MAX_DIST = 128
POOL_WINDOWS = (2, 4, 8, 16)
POOL_GROUP = BRANCH_WIDTH // 4
POOL_PAST = 15
RET_HEADS = 8
RET_HEAD_DIM = BRANCH_WIDTH // RET_HEADS
RET_BLOCK = CHUNK
ROPE_BASE = 10000.0
N_BRANCH = 3
D_FF = 11 * D_MODEL // 4
N_EXPERTS = 8
TOP_K = 2
N_DENSE = (DEPTH + 1) // 2
N_MOE = DEPTH // 2
ALPHA = (2 * DEPTH) ** 0.25
BETA = (8 * DEPTH) ** -0.25
LN_EPS = 1e-5
IN_SPLITS = (BRANCH_WIDTH, BRANCH_WIDTH, BRANCH_WIDTH, IDX_HEADS * IDX_DIM, IDX_DIM, IDX_HEADS,
             BRANCH_WIDTH, BRANCH_WIDTH, BRANCH_WIDTH, BRANCH_WIDTH, BRANCH_WIDTH)
IN_WIDTH = sum(IN_SPLITS)

kernel_name = "hybrid_dsa_pool_retention_stream_step"


def layer_norm(x, g, b):
    xf = x.astype(jnp.float32)
    mu = jnp.mean(xf, -1, keepdims=True)
    var = jnp.mean(jnp.square(xf - mu), -1, keepdims=True)
    return ((xf - mu) * lax.rsqrt(var + LN_EPS) * g + b).astype(x.dtype)


def project(x, w):
    B, T = x.shape[0], x.shape[1]
    h = x @ w
    parts, start = [], 0
    for width in IN_SPLITS:
        parts.append(h[..., start:start + width])
        start += width
    qA, kA, vA, qI, kI, wI, uB, qC, kC, vC, gC = parts
    ad = (B, T, A_HEADS, A_HEAD_DIM)
    rd = (B, T, RET_HEADS, RET_HEAD_DIM)
    f32 = jnp.float32
    return (qA.reshape(ad), kA.reshape(ad), vA.reshape(ad), qI.reshape(B, T, IDX_HEADS, IDX_DIM), kI, wI, uB,
            qC.reshape(rd).astype(f32), kC.reshape(rd).astype(f32), vC.reshape(rd).astype(f32), gC)


def t5_bucket(rel):
    half = T5_BUCKETS // 2
    exact = half // 2
    n = jnp.abs(rel)
    large = exact + (jnp.log(jnp.maximum(n, 1).astype(jnp.float32) / exact)
                     / math.log(T5_MAX_DIST / exact) * (half - exact)).astype(jnp.int32)
    large = jnp.minimum(large, half - 1)
    return jnp.where(rel > 0, half, 0) + jnp.where(n < exact, n, large)


def dsa_attend(q, qi, wi, qpos, k, v, ki, kpos, t5_bias, k_sel):
    B, Tq = q.shape[0], q.shape[1]
    nblk = -(-Tq // QBLK)
    pad = nblk * QBLK - Tq

    def blocks(a):
        a = jnp.pad(a, [(0, 0), (0, pad)] + [(0, 0)] * (a.ndim - 2))
        return a.reshape((B, nblk, QBLK) + a.shape[2:])

    qb, qib, wib = blocks(q), blocks(qi), blocks(wi)
    qposb = jnp.pad(qpos, (0, pad), constant_values=2 ** 30).reshape(nblk, QBLK)
    kchunk = kpos // CHUNK
    scale = A_HEAD_DIM ** -0.5

    def one_block(args):
        q_, qi_, wi_, qp_, k_, v_, ki_ = args
        s = jax.nn.relu(jnp.einsum('thd,sd->ths', qi_, ki_).astype(jnp.float32) * IDX_DIM ** -0.5)
        score = jnp.einsum('th,ths->ts', wi_.astype(jnp.float32), s) * IDX_HEADS ** -0.5
        adm = kchunk[None, :] <= (qp_ // CHUNK)[:, None]
        score = jnp.where(adm, score, -jnp.inf)
        _, idx = lax.top_k(score, k_sel)
        valid = jnp.take_along_axis(adm, idx, axis=1)
        kg, vg = k_[idx], v_[idx]
        bias = t5_bias[t5_bucket(kpos[idx] - qp_[:, None])]
        logits = (jnp.einsum('qhd,qkhd->qhk', q_, kg).astype(jnp.float32) * scale
                  + jnp.transpose(bias, (0, 2, 1)).astype(jnp.float32))
        logits = jnp.where(valid[:, None, :], logits, -jnp.inf)
        p = jax.nn.softmax(logits, axis=-1).astype(v_.dtype)
        return jnp.einsum('qhk,qkhd->qhd', p, vg)

    def one_seq(args):
        q_s, qi_s, wi_s, k_s, v_s, ki_s = args
        return lax.map(lambda a: one_block(a + (k_s, v_s, ki_s)), (q_s, qi_s, wi_s, qposb))

    out = lax.map(one_seq, (qb, qib, wib, k, v, ki))
    return out.reshape(B, nblk * QBLK, A_HEADS * A_HEAD_DIM)[:, :Tq]


def pool_mix(u_ext, valid_ext, pool_w, pool_scale):
    B = u_ext.shape[0]
    T = u_ext.shape[1] - POOL_PAST
    uf = u_ext.astype(jnp.float32)
    cs = jnp.cumsum(jnp.pad(uf, ((0, 0), (1, 0), (0, 0))), axis=1)
    cn = jnp.cumsum(jnp.pad(valid_ext.astype(jnp.float32), (1, 0)))
    end = POOL_PAST + 1
    outs = []
    for g, w in enumerate(POOL_WINDOWS):
        sl = slice(g * POOL_GROUP, (g + 1) * POOL_GROUP)
        s = cs[:, end:end + T, sl] - cs[:, end - w:end - w + T, sl]
        n = cn[end:end + T] - cn[end - w:end - w + T]
        outs.append(s / n[None, :, None] - uf[:, POOL_PAST:, sl])
    d = jnp.stack(outs, axis=2)
    y = jnp.einsum('btgc,gce->btge', d, pool_w.astype(jnp.float32)).reshape(B, T, BRANCH_WIDTH)
    return (y * pool_scale).astype(u_ext.dtype)


def rotary(x, pos):
    half = x.shape[-1] // 2
    inv = ROPE_BASE ** (-jnp.arange(half, dtype=jnp.float32) / half)
    ang = pos.astype(jnp.float32)[:, None] * inv[None, :]
    cos, sin = jnp.cos(ang)[None, :, None, :], jnp.sin(ang)[None, :, None, :]
    x1, x2 = x[..., :half], x[..., half:]
    return jnp.concatenate([x1 * cos - x2 * sin, x1 * sin + x2 * cos], axis=-1)


def ret_chunk(S, q, k, v, log_g):
    n = q.shape[1]
    i = jnp.arange(n, dtype=jnp.float32)
    diff = i[:, None] - i[None, :]
    D = jnp.where(diff >= 0, jnp.exp(jnp.maximum(diff, 0.0)[None] * log_g[:, None, None]), 0.0)
    inner = jnp.einsum('bnhk,bmhk->bhnm', q, k) * D[None]
    cross = jnp.exp((i[None, :] + 1.0) * log_g[:, None])
    o = (jnp.einsum('bhnm,bmhv->bnhv', inner, v)
         + jnp.einsum('bnhk,bhkv->bnhv', q, S) * cross.T[None, :, :, None])
    kdec = jnp.exp((n - 1.0 - i)[None, :] * log_g[:, None])
    S_new = (jnp.exp(n * log_g)[None, :, None, None] * S
             + jnp.einsum('bmhk,bmhv->bhkv', k * kdec.T[None, :, :, None], v))
    return S_new, o


def retention_prompt(q, k, v, log_g):
    B, T = q.shape[0], q.shape[1]
    pad = (-T) % RET_BLOCK
    nb = (T + pad) // RET_BLOCK

    def to_blocks(a):
        a = jnp.pad(a, ((0, 0), (pad, 0), (0, 0), (0, 0)))
        return jnp.transpose(a.reshape(B, nb, RET_BLOCK, RET_HEADS, RET_HEAD_DIM), (1, 0, 2, 3, 4))

    S0 = jnp.zeros((B, RET_HEADS, RET_HEAD_DIM, RET_HEAD_DIM), jnp.float32)
    S, o = lax.scan(lambda S_, xs: ret_chunk(S_, xs[0], xs[1], xs[2], log_g), S0,
                    (to_blocks(q), to_blocks(k), to_blocks(v)))
    o = jnp.transpose(o, (1, 0, 2, 3, 4)).reshape(B, nb * RET_BLOCK, RET_HEADS, RET_HEAD_DIM)[:, pad:]
    return o, S


def retention_output(o, gate):
    B, T = o.shape[0], o.shape[1]
    mu = jnp.mean(o, -1, keepdims=True)
    var = jnp.mean(jnp.square(o - mu), -1, keepdims=True)
    on = ((o - mu) * lax.rsqrt(var + LN_EPS)).reshape(B, T, BRANCH_WIDTH)
    return (jax.nn.silu(gate.astype(jnp.float32)) * on).astype(gate.dtype)


def merge_branches(x, oA, oB, oC, w_branch, w_gate, b_gate, w_out):
    B, T = x.shape[0], x.shape[1]
    ob = jnp.stack([oA, oB.astype(oA.dtype), oC.astype(oA.dtype)], axis=2)
    u = jnp.einsum('btnw,nwd->btnd', ob, w_branch)
    g = jax.nn.sigmoid((x @ w_gate + b_gate).reshape(B, T, N_BRANCH, D_MODEL))
    return jnp.sum(g * u, axis=2) @ w_out


def swiglu(x, wg, wu, wd):
    return (jax.nn.silu(x @ wg) * (x @ wu)) @ wd


def moe(x, w_r, b_r, wg, wu, wd):
    logits = (x @ w_r).astype(jnp.float32) + b_r
    top_v, top_i = lax.top_k(logits, TOP_K)
    probs = jax.nn.softmax(top_v, axis=-1)
    gates = jnp.sum(probs[..., None] * jax.nn.one_hot(top_i, N_EXPERTS, dtype=jnp.float32), axis=-2)
    out = jnp.zeros_like(x)
    for e in range(N_EXPERTS):
        out = out + gates[..., e:e + 1].astype(x.dtype) * swiglu(x, wg[e], wu[e], wd[e])
    return out


def channel_mixer(l, x, ffn_w_gate, ffn_w_up, ffn_w_down, moe_w_router, moe_b_router, moe_w_gate, moe_w_up, moe_w_down):
    i = l // 2
    if l % 2 == 0:
        return swiglu(x, ffn_w_gate[i], ffn_w_up[i], ffn_w_down[i])
    return moe(x, moe_w_router[i], moe_b_router[i], moe_w_gate[i], moe_w_up[i], moe_w_down[i])


def setup_inputs(seed: int = 0) -> dict:
    key = jax.random.key(seed)
    ks = jax.random.split(key, 32)
    f32 = jnp.float32

    def nrm(i, shape, scale):
        return jax.random.normal(ks[i], shape, f32) * scale

    D = D_MODEL
    return {
        "x_prompt": nrm(0, (BATCH, SEQ, D), 1.0),
        "x_sample": nrm(1, (DEC_BATCH, DEC_SEQ, D), 1.0),
        "cache_k": nrm(2, (DEPTH, DEC_BATCH, PAST_LEN, A_HEADS, A_HEAD_DIM), 1.0),
        "cache_v": nrm(3, (DEPTH, DEC_BATCH, PAST_LEN, A_HEADS, A_HEAD_DIM), 1.0),
        "cache_ki": nrm(4, (DEPTH, DEC_BATCH, PAST_LEN, IDX_DIM), 1.0),
        "cache_pool": nrm(5, (DEPTH, DEC_BATCH, POOL_PAST, BRANCH_WIDTH), 1.0),
        "state_ret": nrm(6, (DEPTH, DEC_BATCH, RET_HEADS, RET_HEAD_DIM, RET_HEAD_DIM), 4.0),
        "meta_tokens": nrm(7, (N_META, D), 1.0),
        "ln_in_g": 1.0 + nrm(8, (D,), 0.02),
        "ln_in_b": nrm(9, (D,), 0.02),
        "w_in": nrm(10, (DEPTH, D, IN_WIDTH), D ** -0.5),
        "t5_bias": nrm(11, (T5_BUCKETS, A_HEADS), 0.5),
        "pool_w": nrm(12, (DEPTH, len(POOL_WINDOWS), POOL_GROUP, POOL_GROUP), POOL_GROUP ** -0.5),
        "pool_scale": 1.0 + nrm(13, (DEPTH, BRANCH_WIDTH), 0.1),
        "w_branch": nrm(14, (DEPTH, N_BRANCH, BRANCH_WIDTH, D), BRANCH_WIDTH ** -0.5),
        "w_gate": nrm(15, (DEPTH, D, N_BRANCH * D), D ** -0.5),
        "b_gate": nrm(16, (DEPTH, N_BRANCH * D), 0.02),
        "w_out": nrm(17, (DEPTH, D, D), BETA * D ** -0.5),
        "ln1_g": 1.0 + nrm(18, (DEPTH, D), 0.02),
        "ln1_b": nrm(19, (DEPTH, D), 0.02),
        "ln2_g": 1.0 + nrm(20, (DEPTH, D), 0.02),
        "ln2_b": nrm(21, (DEPTH, D), 0.02),
        "ffn_w_gate": nrm(22, (N_DENSE, D, D_FF), D ** -0.5),
        "ffn_w_up": nrm(23, (N_DENSE, D, D_FF), D ** -0.5),
        "ffn_w_down": nrm(24, (N_DENSE, D_FF, D), BETA * D_FF ** -0.5),
        "moe_w_router": nrm(25, (N_MOE, D, N_EXPERTS), D ** -0.5),
        "moe_b_router": nrm(26, (N_MOE, N_EXPERTS), 0.01),
        "moe_w_gate": nrm(27, (N_MOE, N_EXPERTS, D, D_FF), D ** -0.5),
        "moe_w_up": nrm(28, (N_MOE, N_EXPERTS, D, D_FF), D ** -0.5),
        "moe_w_down": nrm(29, (N_MOE, N_EXPERTS, D_FF, D), BETA * D_FF ** -0.5),
    }


def reference(x_prompt, x_sample, cache_k, cache_v, cache_ki, cache_pool, state_ret,
              meta_tokens, ln_in_g, ln_in_b, w_in, t5_bias, pool_w, pool_scale, w_branch, w_gate, b_gate,
              w_out, ln1_g, ln1_b, ln2_g, ln2_b, ffn_w_gate, ffn_w_up, ffn_w_down,
              moe_w_router, moe_b_router, moe_w_gate, moe_w_up, moe_w_down):
    f32 = jnp.float32
    B, S_len = x_prompt.shape[0], x_prompt.shape[1]
    Bs, Ts = x_sample.shape[0], x_sample.shape[1]
    past = cache_k.shape[2]
    meta = jnp.broadcast_to(meta_tokens.astype(x_prompt.dtype)[None], (B, N_META, D_MODEL))
    xp = layer_norm(jnp.concatenate([meta, x_prompt], axis=1), ln_in_g, ln_in_b)
    xs = layer_norm(x_sample, ln_in_g, ln_in_b)
    T = S_len + N_META
    pos_p = jnp.arange(T, dtype=jnp.int32) - N_META
    pos_s = jnp.arange(past + Ts, dtype=jnp.int32)
    pos_s_new = pos_s[past:]
    ksel_p = min(TOPK_MAX, S_len // 4)
    ksel_s = min(TOPK_MAX, (past + Ts) // 4)
    valid_p = jnp.concatenate([jnp.zeros((POOL_PAST,), f32), jnp.ones((T,), f32)])
    valid_s = jnp.ones((POOL_PAST + Ts,), f32)
    log_g = jnp.log(1.0 - 2.0 ** (-5.0 - jnp.arange(RET_HEADS, dtype=f32)))
    rscale = RET_HEAD_DIM ** -0.5

    kp_l, vp_l, kip_l, poolp_l, retp_l = [], [], [], [], []
    ks_l, vs_l, kis_l, pools_l, rets_l = [], [], [], [], []
    for l in range(DEPTH):
        qA, kA, vA, qI, kI, wI, uB, qC, kC, vC, gC = project(xp, w_in[l])
        oA = dsa_attend(qA, qI, wI, pos_p, kA, vA, kI, pos_p, t5_bias, ksel_p)
        oB = pool_mix(jnp.pad(uB, ((0, 0), (POOL_PAST, 0), (0, 0))), valid_p, pool_w[l], pool_scale[l])
        o_ret, S_p = retention_prompt(rotary(qC, pos_p), rotary(kC, pos_p) * rscale, vC, log_g)
        oC = retention_output(o_ret, gC)
        kp_l.append(kA); vp_l.append(vA); kip_l.append(kI)
        poolp_l.append(uB[:, -POOL_PAST:]); retp_l.append(S_p)
        xp = layer_norm(ALPHA * xp + merge_branches(xp, oA, oB, oC, w_branch[l], w_gate[l], b_gate[l], w_out[l]),
                        ln1_g[l], ln1_b[l])
        xp = layer_norm(ALPHA * xp + channel_mixer(l, xp, ffn_w_gate, ffn_w_up, ffn_w_down, moe_w_router,
                                                   moe_b_router, moe_w_gate, moe_w_up, moe_w_down),
                        ln2_g[l], ln2_b[l])

        qA, kA, vA, qI, kI, wI, uB, qC, kC, vC, gC = project(xs, w_in[l])
        k_all = jnp.concatenate([cache_k[l].astype(kA.dtype), kA], axis=1)
        v_all = jnp.concatenate([cache_v[l].astype(vA.dtype), vA], axis=1)
        ki_all = jnp.concatenate([cache_ki[l].astype(kI.dtype), kI], axis=1)
        oA = dsa_attend(qA, qI, wI, pos_s_new, k_all, v_all, ki_all, pos_s, t5_bias, ksel_s)
        u_ext = jnp.concatenate([cache_pool[l].astype(uB.dtype), uB], axis=1)
        oB = pool_mix(u_ext, valid_s, pool_w[l], pool_scale[l])
        S_s, o_ret = ret_chunk(state_ret[l].astype(f32), rotary(qC, pos_s_new),
                               rotary(kC, pos_s_new) * rscale, vC, log_g)
        oC = retention_output(o_ret, gC)
        ks_l.append(kA); vs_l.append(vA); kis_l.append(kI)
        pools_l.append(u_ext[:, -POOL_PAST:]); rets_l.append(S_s)
        xs = layer_norm(ALPHA * xs + merge_branches(xs, oA, oB, oC, w_branch[l], w_gate[l], b_gate[l], w_out[l]),
                        ln1_g[l], ln1_b[l])
        xs = layer_norm(ALPHA * xs + channel_mixer(l, xs, ffn_w_gate, ffn_w_up, ffn_w_down, moe_w_router,
                                                   moe_b_router, moe_w_gate, moe_w_up, moe_w_down),
                        ln2_g[l], ln2_b[l])

    y_prompt = xp[:, N_META:]
    y_sample = xs
    return (y_prompt, y_sample,
            jnp.stack(kp_l), jnp.stack(vp_l), jnp.stack(kip_l), jnp.stack(poolp_l), jnp.stack(retp_l),
            jnp.stack(ks_l), jnp.stack(vs_l), jnp.stack(kis_l), jnp.stack(pools_l), jnp.stack(rets_l))
```

```python
import numpy as np
import ml_dtypes
import concourse.bass as bass
import concourse.mybir as mybir
from concourse.bass_utils import run_bass_kernel_spmd

F32 = mybir.dt.float32
BF16 = mybir.dt.bfloat16
ALU = mybir.AluOpType
AF = mybir.ActivationFunctionType

D = 2048
NTP = 33
NT = 34
NTOK = NT * 128
NPK = NTP * 128
FRONT = 48
INW = 9296
DFF = 5632
BIG = 30000.0
ALPHA = 4.0 ** 0.25
EPS = 1e-5
C_QA, C_KA, C_VA, C_QI, C_KI, C_WI, C_UB, C_QC, C_KC, C_VC, C_GC = 0, 1024, 2048, 3072, 4096, 4160, 4176, 5200, 6224, 7248, 8272
GAM = [1.0 - 2.0 ** (-5.0 - h) for h in range(8)]
STOP_AFTER = None
USED_INPUTS = set()
DEV_CORES = None
ATT_MODE = 2


class Buf:
    __slots__ = ("w", "r")

    def __init__(self):
        self.w = None
        self.r = []


class Sync:
    EPOCH = 10 ** 9
    NSLOT = 40

    def __init__(self, nc):
        self.nc = nc
        self.eng = {"pe": nc.tensor, "act": nc.scalar, "dve": nc.vector, "pool": nc.gpsimd, "sp": nc.sync}
        self.sem = {}
        self.ep = {}
        self.cnt = {}
        for e in self.eng:
            self.ep[e] = 0
            self.cnt[e] = 0
            self.sem[(e, 0)] = nc.alloc_semaphore(name=f"s_{e}_0")
        self.dsem = [nc.alloc_semaphore(name=f"d_{i}") for i in range(self.NSLOT)]
        self.dcnt = [0] * self.NSLOT
        self.dnext = 0
        self.waited = {e: {} for e in self.eng}
        self.bufs = {}

    def buf(self, *key):
        b = self.bufs.get(key)
        if b is None:
            b = self.bufs[key] = Buf()
        return b

    def _wait(self, e, ev):
        if ev is None:
            return
        key, val = ev
        if key[0] == e and e == "pe":
            return
        if key[0] != "dma" and key[0] == e and key[1] != self.ep[e]:
            return
        if self.waited[e].get(key, 0) >= val:
            return
        sem = self.dsem[key[1]] if key[0] == "dma" else self.sem[key]
        self.eng[e].wait_ge(sem, val)
        self.waited[e][key] = val

    def _deps(self, e, reads, writes):
        for b in reads:
            self._wait(e, b.w)
        for b in writes:
            self._wait(e, b.w)
            for ev in b.r:
                self._wait(e, ev)

    def _commit(self, ev, reads, writes):
        for b in reads:
            b.r.append(ev)
            if len(b.r) > 64:
                b.r = b.r[-64:]
        for b in writes:
            b.w = ev
            b.r = []

    def op(self, e, fn, reads=(), writes=()):
        self._deps(e, reads, writes)
        if self.cnt[e] >= self.EPOCH:
            self.ep[e] += 1
            self.cnt[e] = 0
            self.sem[(e, self.ep[e])] = self.nc.alloc_semaphore(name=f"s_{e}_{self.ep[e]}")
        ins = fn()
        self.cnt[e] += 1
        ins.then_inc(self.sem[(e, self.ep[e])], 1)
        ev = ((e, self.ep[e]), self.cnt[e])
        self._commit(ev, reads, writes)
        return ev

    def dma(self, q, out, in_, reads=(), writes=()):
        self._deps(q, reads, writes)
        s = self.dnext
        self.dnext = (s + 1) % self.NSLOT
        if self.dcnt[s]:
            self._wait(q, (("dma", s), 16 * self.dcnt[s]))
        ins = self.eng[q].dma_start(out=out, in_=in_)
        self.dcnt[s] += 1
        ins.then_inc(self.dsem[s], 16)
        ev = (("dma", s), 16 * self.dcnt[s])
        self._commit(ev, reads, writes)
        return ev

    def barrier(self):
        evs = []
        for e in self.eng:
            if self.cnt[e]:
                evs.append(((e, self.ep[e]), self.cnt[e]))
        for s in range(self.NSLOT):
            if self.dcnt[s]:
                evs.append((("dma", s), 16 * self.dcnt[s]))
        for e in self.eng:
            for ev in evs:
                self._wait(e, ev)

    def finish(self):
        self.barrier()


def _bucket(rel):
    n = np.abs(rel)
    large = 8 + (np.log(np.maximum(n, 1).astype(np.float32) / 8) / np.log(16.0) * 8).astype(np.int32)
    large = np.minimum(large, 15)
    return np.where(rel > 0, 16, 0) + np.where(n < 8, n, large)


def host_consts():
    c = {}
    q = np.arange(128)[:, None]
    j = np.arange(256)[None, :]
    bk = _bucket(j - 128 - q)
    oh = np.zeros((128, 32, 256), np.float32)
    for b in range(32):
        oh[:, b, :] = (bk == b)
    oh[:, 15, :] -= 1.0
    c["c_oh"] = oh.reshape(128, 32 * 256)
    tm = np.ones((NT, 128), np.float32)
    tm[0, :FRONT] = 0
    tm[32, 64:] = 0
    c["c_tmask"] = np.ascontiguousarray(tm.T)
    c["c_ident"] = np.eye(128, dtype=np.float32)
    wins = (2, 4, 8, 16)
    cur = np.zeros((4, 128, 128), np.float32)
    prv = np.zeros((4, 128, 128), np.float32)
    cur0 = np.zeros((4, 128, 128), np.float32)
    sprv = np.zeros((4, 16, 64), np.float32)
    for g, w in enumerate(wins):
        for t in range(128):
            for i in range(w):
                tp = t - i
                if tp >= 0:
                    cur[g, tp, t] += 1.0 / w
                else:
                    prv[g, 128 + tp, t] += 1.0 / w
            cur[g, t, t] -= 1.0
            n = min(w, t - FRONT + 1) if t >= FRONT else 1
            for i in range(w):
                tp = t - i
                if tp >= 0:
                    cur0[g, tp, t] += 1.0 / n
            cur0[g, t, t] -= 1.0
        for t in range(64):
            for i in range(w):
                tp = t - i
                if tp < 0:
                    sprv[g, 15 + tp, t] += 1.0 / w
    c["c_pcur"] = np.ascontiguousarray(cur.transpose(1, 0, 2)).reshape(128, 512)
    c["c_pprv"] = np.ascontiguousarray(prv.transpose(1, 0, 2)).reshape(128, 512)
    c["c_pcur0"] = np.ascontiguousarray(cur0.transpose(1, 0, 2)).reshape(128, 512)
    c["c_psprv"] = np.ascontiguousarray(sprv.transpose(1, 0, 2)).reshape(16, 256)
    pos = np.zeros(NTOK, np.float64)
    pos[:NPK] = np.arange(NPK) - 64
    pos[NPK:NPK + 64] = 2048 + np.arange(64)
    pos[NPK + 64:] = 2048 + np.arange(64)
    inv = 10000.0 ** (-np.arange(64, dtype=np.float64) / 64)
    ang = (pos[:, None].astype(np.float32) * inv[None, :].astype(np.float32)).astype(np.float32)
    c["c_cos"] = np.cos(ang).astype(np.float32)
    c["c_sin"] = np.sin(ang).astype(np.float32)
    nloc = np.arange(NTOK) % 128
    nloc[NPK:] = np.arange(128) % 64
    g = np.array(GAM, np.float64)
    c["c_qdec"] = (g[None, :] ** (nloc[:, None] + 1)).astype(np.float32)
    c["c_kdec"] = ((128 ** -0.5) * g[None, :] ** (-(nloc[:, None] + 1.0))).astype(np.float32)
    tri = (np.arange(128)[:, None] <= np.arange(128)[None, :]).astype(np.float32)
    c["c_tri"] = tri
    return c


IN_SHAPES = {
    "xin": [NTOK, D], "cache_k": [2, 2, 2048, 1024], "cache_v": [2, 2, 2048, 1024], "cache_ki": [2, 2, 2048, 64],
    "cache_pool": [2, 2, 15, 1024], "state_ret": [2, 2, 8, 128, 128], "ln_in_g": [1, D], "ln_in_b": [1, D],
    "w_in": [2, D, INW], "t5_bias": [1, 256], "pool_w": [2, 4, 256, 256], "pool_scale": [2, 1024],
    "w_branch": [2, 3, 1024, D], "w_gate": [2, D, 3 * D], "b_gate": [2, 3 * D], "w_out": [2, D, D],
    "ln1_g": [2, D], "ln1_b": [2, D], "ln2_g": [2, D], "ln2_b": [2, D],
    "ffn_w_gate": [D, DFF], "ffn_w_up": [D, DFF], "ffn_w_down": [DFF, D],
    "moe_w_router": [D, 8], "moe_b_router": [1, 8], "moe_w_gate": [8, D, DFF], "moe_w_up": [8, D, DFF], "moe_w_down": [8, DFF, D],
    "c_oh": [128, 32 * 256], "c_tmask": [128, NT], "c_ident": [128, 128], "c_pcur": [128, 512], "c_pprv": [128, 512],
    "c_pcur0": [128, 512], "c_psprv": [16, 256], "c_cos": [NTOK, 64], "c_sin": [NTOK, 64], "c_qdec": [NTOK, 8],
    "c_kdec": [NTOK, 8], "c_tri": [128, 128],
}
DEBUG_SCRATCH = False
STAGES = None


def build_program():
    from contextlib import ExitStack
    nc = bass.Bass("TRN2", target_bir_lowering=False)
    S = Sync(nc)
    P = {}
    uid = [0]

    def IN(name):
        if name not in P:
            P[name] = nc.dram_tensor(name, list(IN_SHAPES[name]), F32, kind="ExternalInput").ap()
            USED_INPUTS.add(name)
        return P[name]

    def dout(name, shape, dt=F32):
        P[name] = nc.dram_tensor(name, list(shape), dt, kind="ExternalOutput").ap()
        return P[name]

    def dscr(name, shape, dt=F32):
        if DEBUG_SCRATCH:
            return dout(name, shape, dt)
        return nc.dram_tensor(name, list(shape), dt, kind="Internal").ap()

    def SB(es, name, shape, dt=F32):
        uid[0] += 1
        return es.enter_context(nc.sbuf_tensor(f"{name}_{uid[0]}", list(shape), dt))

    def on(stage):
        return STAGES is None or stage in STAGES

    xin = IN("xin")
    o_y = dout("o_y", [NTOK, D])
    o_k = dout("o_k", [2, NTOK, 1024]); o_v = dout("o_v", [2, NTOK, 1024]); o_ki = dout("o_ki", [2, NTOK, 64])
    o_u = dout("o_u", [2, NTOK, 1024])
    o_retp = dout("o_retp", [2, 8, 128, 128]); o_rets = dout("o_rets", [2, 2, 8, 128, 128])

    X = dscr("X", [NTOK, D]); X1 = dscr("X1", [NTOK, D])
    H = dscr("H", [NTOK, INW])
    Mm = dscr("Mm", [NTOK, NPK], BF16)
    OA = dscr("OA", [NTOK, 1024], BF16); OB = dscr("OB", [NTOK, 1024], BF16); OC = dscr("OC", [NTOK, 1024], BF16)
    Z = dscr("Z", [NTOK, D], BF16)

    ps = [nc.alloc_psum_tensor(f"ps{i}", [128, 512], F32) for i in range(8)]
    psb = [S.buf("ps", i) for i in range(8)]
    psi = [0]

    def next_ps():
        i = psi[0]
        psi[0] = (i + 1) % 8
        return ps[i], psb[i]

    acci = [0]

    def next_acc_ps():
        acci[0] ^= 1
        return ps[6 + acci[0]], psb[6 + acci[0]]

    ident_b = nc.alloc_sbuf_tensor("ident_b", [128, 128], BF16)
    ident_f = nc.alloc_sbuf_tensor("ident_f", [128, 128], F32)
    tmask = nc.alloc_sbuf_tensor("tmask", [128, NT], F32)
    ones_b = nc.alloc_sbuf_tensor("ones_b", [1, 128], BF16)
    ones_f = nc.alloc_sbuf_tensor("ones_f", [1, 128], F32)
    Bc = S.buf("consts")
    S.dma("pool", ident_b[:], IN("c_ident")[:, :], writes=[Bc])
    S.dma("sp", ident_f[:], IN("c_ident")[:, :], writes=[Bc])
    S.dma("sp", tmask[:], IN("c_tmask")[:, :], writes=[Bc])
    S.op("dve", lambda: nc.vector.memset(ones_b[:], 1.0), [], [Bc])
    S.op("dve", lambda: nc.vector.memset(ones_f[:], 1.0), [], [Bc])
    evac_rr = [0]

    def evac(out, in_, reads, writes):
        evac_rr[0] ^= 1
        if evac_rr[0]:
            return S.op("act", lambda: nc.scalar.activation(out=out, in_=in_, func=AF.Copy), reads, writes)
        return S.op("dve", lambda: nc.vector.tensor_copy(out=out, in_=in_), reads, writes)

    def rstd_from_var(var_ap, tmp_ap, out_ap, Bst):
        S.op("dve", lambda: nc.vector.tensor_scalar(out=tmp_ap, in0=var_ap, scalar1=EPS, scalar2=None, op0=ALU.add), [Bst], [Bst])
        S.op("act", lambda: nc.scalar.activation(out=tmp_ap, in_=tmp_ap, func=AF.Sqrt), [Bst], [Bst])
        S.op("dve", lambda: nc.vector.reciprocal(out=out_ap, in_=tmp_ap), [Bst], [Bst])

    def ln_tile(src, dst, g_bc, b_bc, stats, mv, Bsrc, Bdst, Bst, mask_col=None):
        for c in range(4):
            S.op("dve", lambda c=c: nc.vector.bn_stats(out=stats[:, c * 6:(c + 1) * 6], in_=src[:, c * 512:(c + 1) * 512]), [Bsrc], [Bst])
        S.op("dve", lambda: nc.vector.bn_aggr(out=mv[:, 0:2], in_=stats[:, 0:24]), [Bst], [Bst])
        rstd_from_var(mv[:, 1:2], mv[:, 3:4], mv[:, 2:3], Bst)
        if mask_col is not None:
            S.op("dve", lambda: nc.vector.tensor_tensor(out=mv[:, 2:3], in0=mv[:, 2:3], in1=mask_col, op=ALU.mult), [Bst, Bc], [Bst])
        S.op("dve", lambda: nc.vector.tensor_scalar(out=dst, in0=src, scalar1=mv[:, 0:1], scalar2=mv[:, 2:3],
                                                     op0=ALU.subtract, op1=ALU.mult), [Bsrc, Bst], [Bdst])
        S.op("dve", lambda: nc.vector.tensor_tensor(out=dst, in0=dst, in1=g_bc, op=ALU.mult), [Bdst, Bc], [Bdst])
        if mask_col is not None:
            S.op("dve", lambda: nc.vector.scalar_tensor_tensor(out=dst, in0=b_bc, scalar=mask_col, in1=dst,
                                                                op0=ALU.mult, op1=ALU.add), [Bdst, Bc], [Bdst])
        else:
            S.op("dve", lambda: nc.vector.tensor_tensor(out=dst, in0=dst, in1=b_bc, op=ALU.add), [Bdst, Bc], [Bdst])

    def mask_of(t):
        return tmask[:, t:t + 1] if t in (0, 32) else None

    def load_T(dst, kofs, Bdst, src, c0, K, tiles, srcbuf, tmpl):
        KC = K // 128
        for j, t in enumerate(tiles):
            tm_, Bt = tmpl[j % 2]
            S.dma("pool", tm_[:, 0:K], src[t * 128:(t + 1) * 128, c0:c0 + K], reads=[S.buf(srcbuf, t)], writes=[Bt])
            for k0 in range(0, KC, 4):
                kn = min(4, KC - k0)
                p_, Bp = next_ps()
                pv = p_[:].bitcast(BF16)
                for k in range(kn):
                    S.op("pe", lambda k=k, pv=pv, tm_=tm_: nc.tensor.transpose(out=pv[:, k * 128:(k + 1) * 128],
                         in_=tm_[:, (k0 + k) * 128:(k0 + k + 1) * 128], identity=ident_b[:]), [Bt, Bc], [Bp])
                evac(dst[:, kofs + k0:kofs + k0 + kn, j * 128:(j + 1) * 128],
                     pv[:, 0:kn * 128].rearrange("p (k n) -> p k n", k=kn), [Bp], [Bdst])

    def chunks(n, c):
        return [list(range(s, min(s + c, n))) for s in range(0, n, c)]

    with ExitStack() as es:
        g_bc = SB(es, "g_bc", [128, D]); b_bc = SB(es, "b_bc", [128, D])
        S.dma("sp", g_bc[:], IN("ln_in_g")[0:1, :].to_broadcast([128, D]), writes=[Bc])
        S.dma("sp", b_bc[:], IN("ln_in_b")[0:1, :].to_broadcast([128, D]), writes=[Bc])
        xt = [SB(es, "xt", [128, D]) for i in range(2)]
        xo = [SB(es, "xo", [128, D]) for i in range(2)]
        st = [SB(es, "st", [128, 24]) for i in range(2)]
        mv = [SB(es, "mv", [128, 4]) for i in range(2)]
        for t in range(NT):
            i = t % 2
            Bx, Bo, Bs_ = S.buf("xt", i), S.buf("xo", i), S.buf("st", i)
            S.dma("sp", xt[i][:], xin[t * 128:(t + 1) * 128, :], writes=[Bx])
            ln_tile(xt[i][:], xo[i][:], g_bc[:], b_bc[:], st[i][:], mv[i][:], Bx, Bo, Bs_, mask_col=tmask[:, t:t + 1])
            S.dma("sp", X[t * 128:(t + 1) * 128, :], xo[i][:], reads=[Bo], writes=[S.buf("X", t)])
        S.barrier()

    def phase_A(l):
        with ExitStack() as es:
            NS = 17
            xT = SB(es, "xT", [128, 16, NS * 128], BF16)
            tmpl = [(SB(es, "tA", [128, D], BF16), S.buf("tA", i)) for i in range(2)]
            wbs = [(SB(es, "wb", [128, 16, 512], BF16), S.buf("wb", i)) for i in range(2)]
            hss = [(SB(es, "hs", [128, 512], F32), S.buf("hs", i)) for i in range(3)]
            BxT = S.buf("xT")
            wi = 0; hi = 0
            w_in = IN("w_in")
            for tiles in chunks(NT, NS):
                load_T(xT, 0, BxT, X, 0, D, tiles, "X", tmpl)
                for c0 in range(0, INW, 512):
                    cw = min(512, INW - c0)
                    wb, Bw = wbs[wi % 2]; wi += 1
                    S.dma("pool", wb[:, :, 0:cw], w_in[l, :, c0:c0 + cw].rearrange("(k p) n -> p k n", p=128), writes=[Bw])
                    for j, t in enumerate(tiles):
                        p_, Bp = next_ps()
                        for k in range(16):
                            S.op("pe", lambda k=k, p_=p_, wb=wb, j=j: nc.tensor.matmul(p_[:, 0:cw], lhsT=xT[:, k, j * 128:(j + 1) * 128],
                                 rhs=wb[:, k, 0:cw], start=(k == 0), stop=(k == 15)), [BxT, Bw], [Bp])
                        hs, Bh = hss[hi % 3]; hi += 1
                        evac(hs[:, 0:cw], p_[:, 0:cw], [Bp], [Bh])
                        S.dma("sp", H[t * 128:(t + 1) * 128, c0:c0 + cw], hs[:, 0:cw], reads=[Bh], writes=[S.buf("H", t)])
            S.barrier()
        allH = [S.buf("H", t) for t in range(NT)]
        S.dma("sp", o_k[l, :, :], H[:, C_KA:C_KA + 1024], reads=allH, writes=[S.buf("out")])
        S.dma("sp", o_v[l, :, :], H[:, C_VA:C_VA + 1024], reads=allH, writes=[S.buf("out")])
        S.dma("sp", o_ki[l, :, :], H[:, C_KI:C_KI + 64], reads=allH, writes=[S.buf("out")])
        S.dma("sp", o_u[l, :, :], H[:, C_UB:C_UB + 1024], reads=allH, writes=[S.buf("out")])
        S.barrier()

    def transpose_into(dst_ap, src_ap, n, m, Bsrc, Bdst, f32=False):
        p_, Bp = next_ps()
        pv = p_[:] if f32 else p_[:].bitcast(BF16)
        idt = ident_f if f32 else ident_b
        S.op("pe", lambda: nc.tensor.transpose(out=pv[0:m, 0:n], in_=src_ap, identity=idt[0:n, 0:n]), [Bsrc, Bc], [Bp])
        evac(dst_ap, pv[0:m, 0:n], [Bp], [Bdst])

    def phase_index(l):
        with ExitStack() as es:
            kIT = SB(es, "kIT", [64, NPK], F32); BkIT = S.buf("kIT")
            kITs = SB(es, "kITs", [64, 2112], F32); BkITs = S.buf("kITs")
            acc = SB(es, "acc", [128, NPK]); Bacc = S.buf("acc")
            wk = SB(es, "wk", [128, NPK]); Bwk = S.buf("wk")
            mts = [(SB(es, "mt", [128, NPK], BF16), S.buf("mt", i)) for i in range(2)]
            qis = [(SB(es, "qi", [128, 1024]), S.buf("qi", i)) for i in range(2)]
            wis = [(SB(es, "wi", [128, 48]), S.buf("wi", i)) for i in range(2)]
            qss = [(SB(es, "qs", [128, 1024], F32), S.buf("qs", i)) for i in range(2)]
            qsTs = [(SB(es, "qsT", [64, 16, 128], F32), S.buf("qsT", i)) for i in range(2)]
            rls = [(SB(es, "rl", [128, 512]), S.buf("rl", i)) for i in range(3)]
            m8 = SB(es, "m8", [128, 16]); Bm8 = S.buf("m8")
            kits = [(SB(es, "kit", [128, 64], F32), S.buf("kit", i)) for i in range(2)]
            cache_ki = IN("cache_ki")
            for t in range(NTP):
                kt_, Bk = kits[t % 2]
                S.dma("sp", kt_[:], H[t * 128:(t + 1) * 128, C_KI:C_KI + 64], reads=[S.buf("H", t)], writes=[Bk])
                transpose_into(kIT[:, t * 128:(t + 1) * 128], kt_[:], 128, 64, Bk, BkIT, f32=True)
            rli = [0]

            def unit(ui, row0, n, kT, BkT, nk, prompt):
                qi, Bqi = qis[ui % 2]; wi_, Bwi = wis[ui % 2]; qs, Bqs = qss[ui % 2]; qsT, BqsT = qsTs[ui % 2]; mt, Bmt = mts[ui % 2]
                tb = S.buf("H", row0 // 128)
                S.dma("sp", qi[0:n, :], H[row0:row0 + n, C_QI:C_QI + 1024], reads=[tb], writes=[Bqi])
                S.dma("sp", wi_[0:n, 0:16], H[row0:row0 + n, C_WI:C_WI + 16], reads=[tb], writes=[Bwi])
                S.op("act", lambda: nc.scalar.activation(out=wi_[0:n, 16:32], in_=wi_[0:n, 0:16], func=AF.Abs), [Bwi], [Bwi])
                S.op("dve", lambda: nc.vector.tensor_scalar(out=wi_[0:n, 32:48], in0=wi_[0:n, 0:16], scalar1=0.0, scalar2=2.0, op0=ALU.is_gt, op1=ALU.mult), [Bwi], [Bwi])
                S.op("dve", lambda: nc.vector.tensor_scalar(out=wi_[0:n, 32:48], in0=wi_[0:n, 32:48], scalar1=-1.0, scalar2=None, op0=ALU.add), [Bwi], [Bwi])
                S.op("dve", lambda: nc.vector.tensor_tensor(out=qs[0:n, :].rearrange("p (h d) -> p h d", h=16), in0=qi[0:n, :].rearrange("p (h d) -> p h d", h=16),
                     in1=wi_[0:n, 16:32].unsqueeze(2).to_broadcast([n, 16, 64]), op=ALU.mult), [Bqi, Bwi], [Bqs])
                for h0 in range(0, 16, 4):
                    p_, Bp = next_ps()
                    pv = p_[:]
                    for k in range(4):
                        S.op("pe", lambda k=k, pv=pv: nc.tensor.transpose(out=pv[0:64, k * 128:k * 128 + n], in_=qs[0:n, (h0 + k) * 64:(h0 + k + 1) * 64],
                             identity=ident_f[0:n, 0:n]), [Bqs, Bc], [Bp])
                    evac(qsT[:, h0:h0 + 4, 0:n], pv[0:64, 0:512].rearrange("p (k n) -> p k n", k=4)[:, :, 0:n], [Bp], [BqsT])
                for kb in range(0, nk, 512):
                    w = min(512, nk - kb)
                    for h in range(16):
                        p_, Bp = next_ps()
                        S.op("pe", lambda h=h, p_=p_: nc.tensor.matmul(p_[0:n, 0:w], lhsT=qsT[:, h, 0:n], rhs=kT[:, kb:kb + w], start=True, stop=True),
                             [BqsT, BkT], [Bp])
                        rl, Brl = rls[rli[0] % 3]; rli[0] += 1
                        S.op("act", lambda p_=p_, rl=rl: nc.scalar.activation(out=rl[0:n, 0:w], in_=p_[0:n, 0:w], func=AF.Relu), [Bp], [Brl])
                        if h == 0:
                            S.op("dve", lambda rl=rl: nc.vector.tensor_scalar(out=acc[0:n, kb:kb + w], in0=rl[0:n, 0:w], scalar1=wi_[0:n, 32:33], scalar2=None,
                                 op0=ALU.mult), [Brl, Bwi], [Bacc])
                        else:
                            S.op("dve", lambda rl=rl, h=h: nc.vector.scalar_tensor_tensor(out=acc[0:n, kb:kb + w], in0=rl[0:n, 0:w], scalar=wi_[0:n, 32 + h:33 + h],
                                 in1=acc[0:n, kb:kb + w], op0=ALU.mult, op1=ALU.add), [Brl, Bwi, Bacc], [Bacc])
                if prompt:
                    S.op("dve", lambda: nc.vector.memset(acc[0:n, 0:FRONT], -BIG), [], [Bacc])
                    S.op("dve", lambda: nc.vector.memset(acc[0:64, nk - 64:nk], -BIG), [], [Bacc])
                for c0 in range(0, nk, 2048):
                    c1 = min(nk, c0 + 2048)
                    S.op("act", lambda c0=c0, c1=c1: nc.scalar.activation(out=wk[0:n, c0:c1], in_=acc[0:n, c0:c1], func=AF.Copy), [Bacc], [Bwk])
                for r in range(32):
                    S.op("dve", lambda: nc.vector.max(out=m8[0:n, 0:8], in_=wk[0:n, 0:nk]), [Bwk], [Bm8])
                    if r < 31:
                        S.op("dve", lambda: nc.vector.match_replace(out=wk[0:n, 0:nk], in_to_replace=m8[0:n, 0:8], in_values=wk[0:n, 0:nk], imm_value=-BIG),
                             [Bwk, Bm8], [Bwk])
                S.op("dve", lambda: nc.vector.tensor_scalar(out=m8[0:n, 8:9], in0=m8[0:n, 7:8], scalar1=-BIG / 2, scalar2=None, op0=ALU.max), [Bm8], [Bm8])
                for c0 in range(0, nk, 2048):
                    c1 = min(nk, c0 + 2048)
                    S.op("dve", lambda c0=c0, c1=c1: nc.vector.tensor_scalar(out=mt[0:n, c0:c1], in0=acc[0:n, c0:c1], scalar1=m8[0:n, 8:9], scalar2=-BIG, op0=ALU.is_lt, op1=ALU.mult),
                         [Bacc, Bm8], [Bmt])
                S.dma("sp", Mm[row0:row0 + n, 0:nk], mt[0:n, 0:nk], reads=[Bmt], writes=[S.buf("Mm", row0 // 64)])

            for t in range(NTP):
                unit(t, t * 128, 128, kIT, BkIT, 128 * (t + 1), True)
            for s in range(2):
                for t in range(16):
                    kt_, Bk = kits[t % 2]
                    S.dma("sp", kt_[:], cache_ki[l, s, t * 128:(t + 1) * 128, :], writes=[Bk])
                    transpose_into(kITs[:, t * 128:(t + 1) * 128], kt_[:], 128, 64, Bk, BkITs, f32=True)
                kt_, Bk = kits[0]
                r0 = NPK + 64 * s
                S.dma("sp", kt_[0:64, :], H[r0:r0 + 64, C_KI:C_KI + 64], reads=[S.buf("H", 33)], writes=[Bk])
                transpose_into(kITs[:, 2048:2112], kt_[0:64, :], 64, 64, Bk, BkITs, f32=True)
                unit(NTP + s, r0, 64, kITs, BkITs, 2112, False)
            S.barrier()

    def phase_attn(l):
        scale = 128 ** -0.5
        with ExitStack() as es:
            NB = SB(es, "NB", [128, 8, 256]); BNB = S.buf("NB")
            TB = SB(es, "TB", [128, 256])
            ohb = [SB(es, "ohb", [128, 256]) for _ in range(2)]
            S.dma("sp", TB[:], IN("t5_bias")[0:1, :].to_broadcast([128, 256]), writes=[S.buf("TB")])
            S.op("dve", lambda: nc.vector.memset(NB[:], 0.0), [], [BNB])
            for b in range(32):
                o_, Bo = ohb[b % 2], S.buf("ohb", b % 2)
                S.dma("sp", o_[:], IN("c_oh")[:, b * 256:(b + 1) * 256], writes=[Bo])
                for h in range(8):
                    S.op("dve", lambda h=h, o_=o_, b=b: nc.vector.scalar_tensor_tensor(out=NB[:, h, :], in0=o_[:], scalar=TB[:, b * 8 + h:b * 8 + h + 1],
                         in1=NB[:, h, :], op0=ALU.mult, op1=ALU.add), [Bo, S.buf("TB"), BNB], [BNB])
            kT = SB(es, "kT", [128, NPK], BF16); BkT = S.buf("kT")
            qT = SB(es, "qT", [128, NTOK], BF16); BqT = S.buf("qT")
            V = SB(es, "V", [128, 33, 132], BF16); BV = S.buf("V")
            kTs = SB(es, "kTs", [128, 2112], BF16); BkTs = S.buf("kTs")
            Vs = SB(es, "Vs", [128, 17, 132], BF16); BVs = S.buf("Vs")
            tqs = [(SB(es, "tq", [128, 128], BF16), S.buf("tq", i)) for i in range(3)]
            mts = [(SB(es, "amt", [128, NPK], BF16), S.buf("amt", i)) for i in range(2)]
            Ls = [(SB(es, "L", [128, NPK]), S.buf("L", i)) for i in range(2)]
            Ps = [(SB(es, "P", [128, NPK], BF16), S.buf("P", i)) for i in range(2)]
            PTs = [(SB(es, "PT", [128, 8, 128], BF16), S.buf("PT", i)) for i in range(3)]
            obs = [(SB(es, "ob", [128, 128], BF16), S.buf("ob", i)) for i in range(2)]
            rvs = [(SB(es, "rv", [128, 2]), S.buf("rv", i)) for i in range(2)]
            cache_k = IN("cache_k"); cache_v = IN("cache_v")
            S.op("dve", lambda: nc.vector.memset(V[:, :, 128:132], 1.0), [], [BV])
            S.op("dve", lambda: nc.vector.memset(Vs[:, :, 128:132], 1.0), [], [BVs])
            cnt = [0]

            def attend(n, q_ap, kT_, BkT_, V_, BV_, nk, mrow0, nb_lo, nb_ap, orow0, h):
                u = cnt[0]; cnt[0] += 1
                mt, Bmt = mts[u % 2]; L, BL = Ls[u % 2]; Pm, BP = Ps[u % 2]; ob, Bob = obs[u % 2]; rv, Brv = rvs[u % 2]
                S.dma("sp", mt[0:n, 0:nk], Mm[mrow0:mrow0 + n, 0:nk], reads=[S.buf("Mm", mrow0 // 64)], writes=[Bmt])
                for kb in range(0, nk, 512):
                    w = min(512, nk - kb)
                    p_, Bp = next_ps()
                    S.op("pe", lambda p_=p_: nc.tensor.matmul(p_[0:n, 0:w], lhsT=q_ap, rhs=kT_[:, kb:kb + w], start=True, stop=True), [BqT, BkT_], [Bp])
                    S.op("dve", lambda p_=p_: nc.vector.scalar_tensor_tensor(out=L[0:n, kb:kb + w], in0=p_[0:n, 0:w], scalar=scale, in1=mt[0:n, kb:kb + w],
                         op0=ALU.mult, op1=ALU.add), [Bp, Bmt], [BL])
                nbw = nb_ap.shape[-1]
                S.op("dve", lambda: nc.vector.tensor_tensor(out=L[0:n, nb_lo:nb_lo + nbw], in0=L[0:n, nb_lo:nb_lo + nbw], in1=nb_ap, op=ALU.add), [BL, BNB], [BL])
                if ATT_MODE == 0:
                    return
                for c0 in range(0, nk, 2048):
                    c1 = min(nk, c0 + 2048)
                    S.op("act", lambda c0=c0, c1=c1: nc.scalar.activation(out=Pm[0:n, c0:c1], in_=L[0:n, c0:c1], func=AF.Exp), [BL], [BP])
                if ATT_MODE == 1:
                    return
                po, Bpo = next_ps()
                nkt = (nk + 127) // 128
                for k0 in range(0, nkt, 8):
                    kn = min(8, nkt - k0)
                    p_, Bp = next_ps()
                    pv = p_[:].bitcast(BF16)
                    kws = []
                    for k in range(kn):
                        kw = min(128, nk - (k0 + k) * 128)
                        kws.append(kw)
                        S.op("pe", lambda k=k, kw=kw, pv=pv: nc.tensor.transpose(out=pv[0:kw, k * 128:k * 128 + n], in_=Pm[0:n, (k0 + k) * 128:(k0 + k) * 128 + kw],
                             identity=ident_b[0:n, 0:n]), [BP, Bc], [Bp])
                    PT, BPT = PTs[(u + k0 // 8) % 3]
                    if min(kws) == 128:
                        evac(PT[:, 0:kn, 0:n], pv[:, 0:1024].rearrange("p (k n) -> p k n", k=8)[:, 0:kn, 0:n], [Bp], [BPT])
                    else:
                        for k in range(kn):
                            evac(PT[0:kws[k], k, 0:n], pv[0:kws[k], k * 128:k * 128 + n], [Bp], [BPT])
                    for k in range(kn):
                        kt = k0 + k
                        S.op("pe", lambda k=k, kt=kt, PT=PT: nc.tensor.matmul(po[0:n, 0:132], lhsT=PT[0:kws[k], k, 0:n], rhs=V_[0:kws[k], kt, 0:132],
                             start=(kt == 0), stop=(kt == nkt - 1)), [BPT, BV_], [Bpo])
                S.op("dve", lambda: nc.vector.reciprocal(out=rv[0:n, 0:1], in_=po[0:n, 128:129]), [Bpo], [Brv])
                S.op("dve", lambda: nc.vector.tensor_scalar(out=ob[0:n, :], in0=po[0:n, 0:128], scalar1=rv[0:n, 0:1], scalar2=None, op0=ALU.mult), [Bpo, Brv], [Bob])
                S.dma("sp", OA[orow0:orow0 + n, h * 128:(h + 1) * 128], ob[0:n, :], reads=[Bob], writes=[S.buf("OA", orow0 // 128)])

            for h in range(8):
                for t in range(NT):
                    tq, Bt = tqs[t % 3]
                    S.dma("pool", tq[:], H[t * 128:(t + 1) * 128, C_QA + h * 128:C_QA + (h + 1) * 128], reads=[S.buf("H", t)], writes=[Bt])
                    transpose_into(qT[:, t * 128:(t + 1) * 128], tq[:], 128, 128, Bt, BqT)
                for t in range(NTP):
                    tq, Bt = tqs[t % 3]
                    S.dma("pool", tq[:], H[t * 128:(t + 1) * 128, C_KA + h * 128:C_KA + (h + 1) * 128], reads=[S.buf("H", t)], writes=[Bt])
                    transpose_into(kT[:, t * 128:(t + 1) * 128], tq[:], 128, 128, Bt, BkT)
                for t0 in range(0, NTP, 8):
                    t1 = min(NTP, t0 + 8)
                    S.dma("pool", V[:, t0:t1, 0:128], H[t0 * 128:t1 * 128, C_VA + h * 128:C_VA + (h + 1) * 128].rearrange("(t p) d -> p t d", p=128),
                          reads=[S.buf("H", t) for t in range(t0, t1)], writes=[BV])
                for t in range(NTP):
                    nk = 128 * (t + 1)
                    if t == 0:
                        attend(128, qT[:, 0:128], kT, BkT, V, BV, nk, 0, 0, NB[:, h, 128:256], 0, h)
                    else:
                        attend(128, qT[:, t * 128:(t + 1) * 128], kT, BkT, V, BV, nk, t * 128, nk - 256, NB[:, h, :], t * 128, h)
                for s in range(2):
                    for t in range(16):
                        tq, Bt = tqs[t % 3]
                        S.dma("pool", tq[:], cache_k[l, s, t * 128:(t + 1) * 128, h * 128:(h + 1) * 128], writes=[Bt])
                        transpose_into(kTs[:, t * 128:(t + 1) * 128], tq[:], 128, 128, Bt, BkTs)
                    r0 = NPK + 64 * s
                    tq, Bt = tqs[0]
                    S.dma("pool", tq[0:64, :], H[r0:r0 + 64, C_KA + h * 128:C_KA + (h + 1) * 128], reads=[S.buf("H", 33)], writes=[Bt])
                    transpose_into(kTs[:, 2048:2112], tq[0:64, :], 64, 128, Bt, BkTs)
                    for t0 in (0, 8):
                        S.dma("pool", Vs[:, t0:t0 + 8, 0:128], cache_v[l, s, t0 * 128:(t0 + 8) * 128, h * 128:(h + 1) * 128].rearrange("(t p) d -> p t d", p=128), writes=[BVs])
                    S.dma("pool", Vs[0:64, 16, 0:128], H[r0:r0 + 64, C_VA + h * 128:C_VA + (h + 1) * 128], reads=[S.buf("H", 33)], writes=[BVs])
                    attend(64, qT[:, r0:r0 + 64], kTs, BkTs, Vs, BVs, 2112, r0, 1920, NB[0:64, h, 0:192], r0, h)
            S.barrier()

    def phase_pool(l):
        with ExitStack() as es:
            bands = SB(es, "bands", [128, 3, 512], BF16); Bb = S.buf("bands")
            sprv = SB(es, "sprv", [16, 256], BF16)
            pw = SB(es, "pw", [128, 8, 256], BF16)
            psc = SB(es, "psc", [128, 1024])
            us = [(SB(es, "u", [128, 1024], BF16), S.buf("u", i)) for i in range(3)]
            dTs = [(SB(es, "dT", [128, 8, 128], BF16), S.buf("dT", i)) for i in range(2)]
            obs = [(SB(es, "pob", [128, 1024], BF16), S.buf("pob", i)) for i in range(2)]
            S.dma("pool", bands[:, 0, :], IN("c_pcur")[:, :], writes=[Bb])
            S.dma("pool", bands[:, 1, :], IN("c_pprv")[:, :], writes=[Bb])
            S.dma("pool", bands[:, 2, :], IN("c_pcur0")[:, :], writes=[Bb])
            S.dma("pool", sprv[:], IN("c_psprv")[:, :], writes=[Bb])
            S.dma("pool", pw[:], IN("pool_w")[l].rearrange("g (cc p) e -> p (g cc) e", p=128), writes=[Bb])
            S.dma("sp", psc[:], IN("pool_scale")[l:l + 1, :].to_broadcast([128, 1024]), writes=[Bb])
            cache_pool = IN("cache_pool")
            ui = [0]

            def unit(row0, n, u, Bu, up, Bup, npv, cur_i, prv_ap_fn):
                i = ui[0]; ui[0] += 1
                dT, BdT = dTs[i % 2]; ob, Bob = obs[i % 2]
                for g in range(4):
                    for cc in range(2):
                        c0 = g * 256 + cc * 128
                        p_, Bp = next_ps()
                        S.op("pe", lambda p_=p_, c0=c0, g=g: nc.tensor.matmul(p_[:, 0:n], lhsT=u[0:n, c0:c0 + 128], rhs=bands[0:n, cur_i, g * 128:g * 128 + n],
                             start=True, stop=(up is None)), [Bu, Bb], [Bp])
                        if up is not None:
                            S.op("pe", lambda p_=p_, c0=c0, g=g: nc.tensor.matmul(p_[:, 0:n], lhsT=up[0:npv, c0:c0 + 128], rhs=prv_ap_fn(g),
                                 start=False, stop=True), [Bup, Bb], [Bp])
                        evac(dT[:, g * 2 + cc, 0:n], p_[:, 0:n], [Bp], [BdT])
                for half in range(2):
                    p_, Bp = next_ps()
                    for gg in range(2):
                        g = half * 2 + gg
                        for cc in range(2):
                            S.op("pe", lambda p_=p_, g=g, gg=gg, cc=cc: nc.tensor.matmul(p_[0:n, gg * 256:(gg + 1) * 256], lhsT=dT[:, g * 2 + cc, 0:n],
                                 rhs=pw[:, g * 2 + cc, :], start=(cc == 0), stop=(cc == 1)), [BdT, Bb], [Bp])
                    S.op("dve", lambda p_=p_, half=half: nc.vector.tensor_tensor(out=ob[0:n, half * 512:(half + 1) * 512], in0=p_[0:n, :],
                         in1=psc[0:n, half * 512:(half + 1) * 512], op=ALU.mult), [Bp, Bb], [Bob])
                S.dma("sp", OB[row0:row0 + n, :], ob[0:n, :], reads=[Bob], writes=[S.buf("OB", row0 // 128)])

            prev = None
            for t in range(NTP):
                u, Bu = us[t % 3]
                S.dma("pool", u[:], H[t * 128:(t + 1) * 128, C_UB:C_UB + 1024], reads=[S.buf("H", t)], writes=[Bu])
                if t == 0:
                    unit(0, 128, u, Bu, None, None, 0, 2, None)
                else:
                    unit(t * 128, 128, u, Bu, prev[0], prev[1], 128, 0, lambda g: bands[:, 1, g * 128:(g + 1) * 128])
                prev = (u, Bu)
            for s in range(2):
                r0 = NPK + 64 * s
                u, Bu = us[(2 * s) % 3]; up, Bup = us[(2 * s + 1) % 3]
                S.dma("pool", u[0:64, :], H[r0:r0 + 64, C_UB:C_UB + 1024], reads=[S.buf("H", 33)], writes=[Bu])
                S.dma("pool", up[0:15, :], cache_pool[l, s, :, :], writes=[Bup])
                unit(r0, 64, u, Bu, up, Bup, 15, 0, lambda g: sprv[0:15, g * 64:(g + 1) * 64])
            S.barrier()

    def phase_ret(l):
        with ExitStack() as es:
            Sst = SB(es, "Sst", [128, 8, 128]); BS = S.buf("Sst")
            Sbf = SB(es, "Sbf", [128, 8, 128], BF16); BSb = S.buf("Sbf")
            tri = SB(es, "tri", [128, 128]); Btri = S.buf("tri")
            S.dma("sp", tri[:], IN("c_tri")[:, :], writes=[Btri])
            qcs = [(SB(es, "qc", [128, 1024]), S.buf("qc", i)) for i in range(2)]
            kcs = [(SB(es, "kc", [128, 1024]), S.buf("kc", i)) for i in range(2)]
            gcs = [(SB(es, "gc", [128, 1024]), S.buf("gc", i)) for i in range(2)]
            vbs = [(SB(es, "vb", [128, 1024], BF16), S.buf("vb", i)) for i in range(2)]
            css = [(SB(es, "cs", [128, 144]), S.buf("cs", i)) for i in range(2)]
            rq = SB(es, "rq", [128, 1024]); Brq = S.buf("rq")
            tmp = SB(es, "rtmp", [128, 512]); Btmp = S.buf("rtmp")
            qb = SB(es, "qb", [128, 1024], BF16); Bqb = S.buf("qb")
            kb_ = SB(es, "kb", [128, 1024], BF16); Bkb = S.buf("kb")
            qTt = SB(es, "qTt", [128, 8, 128], BF16); BqTt = S.buf("qTt")
            kTt = SB(es, "kTt", [128, 8, 128], BF16); BkTt = S.buf("kTt")
            WTs = [(SB(es, "WT", [128, 128], BF16), S.buf("WT", i)) for i in range(3)]
            osb = SB(es, "osb", [128, 1024]); Bosb = S.buf("osb")
            stt = SB(es, "rstt", [128, 8, 6]); mvv = SB(es, "rmv", [128, 8, 4]); Bst = S.buf("rst")
            ocb = [(SB(es, "ocb", [128, 1024], BF16), S.buf("ocb", i)) for i in range(2)]
            c_cos, c_sin, c_qdec, c_kdec = IN("c_cos"), IN("c_sin"), IN("c_qdec"), IN("c_kdec")
            state_ret = IN("state_ret")

            def rotary(src, n, cs, dst, Bsrc, Bcs, Bdst):
                s4 = src[0:n, :].rearrange("p (h two d) -> p h two d", h=8, two=2)
                d4 = dst[0:n, :].rearrange("p (h two d) -> p h two d", h=8, two=2)
                cosb = cs[0:n, 0:64].unsqueeze(1).to_broadcast([n, 8, 64])
                sinb = cs[0:n, 64:128].unsqueeze(1).to_broadcast([n, 8, 64])
                t3 = tmp[0:n, :].rearrange("p (h d) -> p h d", h=8)
                S.op("dve", lambda: nc.vector.tensor_tensor(out=d4[:, :, 0, :], in0=s4[:, :, 0, :], in1=cosb, op=ALU.mult), [Bsrc, Bcs], [Bdst])
                S.op("dve", lambda: nc.vector.tensor_tensor(out=t3, in0=s4[:, :, 1, :], in1=sinb, op=ALU.mult), [Bsrc, Bcs], [Btmp])
                S.op("dve", lambda: nc.vector.tensor_tensor(out=d4[:, :, 0, :], in0=d4[:, :, 0, :], in1=t3, op=ALU.subtract), [Bdst, Btmp], [Bdst])
                S.op("dve", lambda: nc.vector.tensor_tensor(out=d4[:, :, 1, :], in0=s4[:, :, 0, :], in1=sinb, op=ALU.mult), [Bsrc, Bcs], [Bdst])
                S.op("dve", lambda: nc.vector.tensor_tensor(out=t3, in0=s4[:, :, 1, :], in1=cosb, op=ALU.mult), [Bsrc, Bcs], [Btmp])
                S.op("dve", lambda: nc.vector.tensor_tensor(out=d4[:, :, 1, :], in0=d4[:, :, 1, :], in1=t3, op=ALU.add), [Bdst, Btmp], [Bdst])

            ui = [0]

            def unit(row0, n, gpow):
                i = ui[0]; ui[0] += 1
                qc, Bqc = qcs[i % 2]; kc, Bkc = kcs[i % 2]; gc, Bgc = gcs[i % 2]; vb, Bvb = vbs[i % 2]; cs, Bcs = css[i % 2]
                oc, Boc = ocb[i % 2]
                tb = S.buf("H", row0 // 128)
                S.dma("sp", qc[0:n, :], H[row0:row0 + n, C_QC:C_QC + 1024], reads=[tb], writes=[Bqc])
                S.dma("sp", kc[0:n, :], H[row0:row0 + n, C_KC:C_KC + 1024], reads=[tb], writes=[Bkc])
                S.dma("sp", gc[0:n, :], H[row0:row0 + n, C_GC:C_GC + 1024], reads=[tb], writes=[Bgc])
                S.dma("pool", vb[0:n, :], H[row0:row0 + n, C_VC:C_VC + 1024], reads=[tb], writes=[Bvb])
                S.dma("sp", cs[0:n, 0:64], c_cos[row0:row0 + n, :], writes=[Bcs])
                S.dma("sp", cs[0:n, 64:128], c_sin[row0:row0 + n, :], writes=[Bcs])
                S.dma("sp", cs[0:n, 128:136], c_qdec[row0:row0 + n, :], writes=[Bcs])
                S.dma("sp", cs[0:n, 136:144], c_kdec[row0:row0 + n, :], writes=[Bcs])
                rotary(qc, n, cs, rq, Bqc, Bcs, Brq)
                S.op("dve", lambda: nc.vector.tensor_tensor(out=qb[0:n, :].rearrange("p (h d) -> p h d", h=8), in0=rq[0:n, :].rearrange("p (h d) -> p h d", h=8),
                     in1=cs[0:n, 128:136].unsqueeze(2).to_broadcast([n, 8, 128]), op=ALU.mult), [Brq, Bcs], [Bqb])
                rotary(kc, n, cs, rq, Bkc, Bcs, Brq)
                S.op("dve", lambda: nc.vector.tensor_tensor(out=kb_[0:n, :].rearrange("p (h d) -> p h d", h=8), in0=rq[0:n, :].rearrange("p (h d) -> p h d", h=8),
                     in1=cs[0:n, 136:144].unsqueeze(2).to_broadcast([n, 8, 128]), op=ALU.mult), [Brq, Bcs], [Bkb])
                for src, Bsrc, dstT, BdstT in ((qb, Bqb, qTt, BqTt), (kb_, Bkb, kTt, BkTt)):
                    for h0 in range(0, 8, 4):
                        p_, Bp = next_ps()
                        pv = p_[:].bitcast(BF16)
                        for k in range(4):
                            S.op("pe", lambda k=k, pv=pv, src=src: nc.tensor.transpose(out=pv[:, k * 128:k * 128 + n], in_=src[0:n, (h0 + k) * 128:(h0 + k + 1) * 128],
                                 identity=ident_b[0:n, 0:n]), [Bsrc, Bc], [Bp])
                        evac(dstT[:, h0:h0 + 4, 0:n], pv[:, 0:512].rearrange("p (k n) -> p k n", k=4)[:, :, 0:n], [Bp], [BdstT])
                for hh in range(2):
                    po, Bpo = next_ps()
                    for k in range(4):
                        h = hh * 4 + k
                        p_, Bp = next_ps()
                        S.op("pe", lambda p_=p_, h=h: nc.tensor.matmul(p_[0:n, 0:n], lhsT=kTt[:, h, 0:n], rhs=qTt[:, h, 0:n], start=True, stop=True), [BkTt, BqTt], [Bp])
                        WT, BWT = WTs[h % 3]
                        S.op("dve", lambda p_=p_, WT=WT: nc.vector.tensor_tensor(out=WT[0:n, 0:n], in0=p_[0:n, 0:n], in1=tri[0:n, 0:n], op=ALU.mult), [Bp, Btri], [BWT])
                        S.op("pe", lambda h=h, k=k, WT=WT, po=po: nc.tensor.matmul(po[0:n, k * 128:(k + 1) * 128], lhsT=WT[0:n, 0:n], rhs=vb[0:n, h * 128:(h + 1) * 128],
                             start=True, stop=False), [BWT, Bvb], [Bpo])
                        S.op("pe", lambda h=h, k=k, po=po: nc.tensor.matmul(po[0:n, k * 128:(k + 1) * 128], lhsT=qTt[:, h, 0:n], rhs=Sbf[:, h, :],
                             start=False, stop=True), [BqTt, BSb], [Bpo])
                    evac(osb[0:n, hh * 512:(hh + 1) * 512], po[0:n, :], [Bpo], [Bosb])
                for h in range(8):
                    p_, Bp = next_ps()
                    S.op("pe", lambda p_=p_, h=h: nc.tensor.matmul(p_[:, 0:128], lhsT=kb_[0:n, h * 128:(h + 1) * 128], rhs=vb[0:n, h * 128:(h + 1) * 128],
                         start=True, stop=True), [Bkb, Bvb], [Bp])
                    g = float(GAM[h] ** gpow)
                    S.op("dve", lambda h=h, g=g: nc.vector.tensor_scalar(out=Sst[:, h, :], in0=Sst[:, h, :], scalar1=g, scalar2=None, op0=ALU.mult), [BS], [BS])
                    S.op("dve", lambda h=h, g=g, p_=p_: nc.vector.scalar_tensor_tensor(out=Sst[:, h, :], in0=p_[:, 0:128], scalar=g, in1=Sst[:, h, :],
                         op0=ALU.mult, op1=ALU.add), [Bp, BS], [BS])
                S.op("act", lambda: nc.scalar.activation(out=Sbf[:], in_=Sst[:], func=AF.Copy), [BS], [BSb])
                for h in range(8):
                    S.op("dve", lambda h=h: nc.vector.bn_stats(out=stt[0:n, h, :], in_=osb[0:n, h * 128:(h + 1) * 128]), [Bosb], [Bst])
                    S.op("dve", lambda h=h: nc.vector.bn_aggr(out=mvv[0:n, h, 0:2], in_=stt[0:n, h, :]), [Bst], [Bst])
                rstd_from_var(mvv[0:n, :, 1:2], mvv[0:n, :, 3:4], mvv[0:n, :, 2:3], Bst)
                for h in range(8):
                    S.op("dve", lambda h=h: nc.vector.tensor_scalar(out=osb[0:n, h * 128:(h + 1) * 128], in0=osb[0:n, h * 128:(h + 1) * 128],
                         scalar1=mvv[0:n, h, 0:1], scalar2=mvv[0:n, h, 2:3], op0=ALU.subtract, op1=ALU.mult), [Bosb, Bst], [Bosb])
                S.op("act", lambda: nc.scalar.activation(out=gc[0:n, :], in_=gc[0:n, :], func=AF.Silu), [Bgc], [Bgc])
                S.op("dve", lambda: nc.vector.tensor_tensor(out=oc[0:n, :], in0=osb[0:n, :], in1=gc[0:n, :], op=ALU.mult), [Bosb, Bgc], [Boc])
                S.dma("sp", OC[row0:row0 + n, :], oc[0:n, :], reads=[Boc], writes=[S.buf("OC", row0 // 128)])

            S.op("dve", lambda: nc.vector.memset(Sst[:], 0.0), [], [BS])
            S.op("dve", lambda: nc.vector.memset(Sbf[:], 0.0), [], [BSb])
            for t in range(NTP):
                unit(t * 128, 128, 128 if t < 32 else 64)
            S.dma("sp", o_retp[l].rearrange("h k v -> k h v"), Sst[:], reads=[BS], writes=[S.buf("out")])
            for s in range(2):
                S.dma("sp", Sst[:], state_ret[l, s].rearrange("h k v -> k h v"), writes=[BS])
                S.op("act", lambda: nc.scalar.activation(out=Sbf[:], in_=Sst[:], func=AF.Copy), [BS], [BSb])
                unit(NPK + 64 * s, 64, 64)
                S.dma("sp", o_rets[l, s].rearrange("h k v -> k h v"), Sst[:], reads=[BS], writes=[S.buf("out")])
            S.barrier()

    def phase_merge(l):
        w_gate, b_gate, w_branch, w_out = IN("w_gate"), IN("b_gate"), IN("w_branch"), IN("w_out")
        NS = 6
        with ExitStack() as es:
            xT = SB(es, "cxT", [128, 40, NS * 128], BF16); BxT = S.buf("cxT")
            tmpl = [(SB(es, "ctA", [128, D], BF16), S.buf("ctA", i)) for i in range(2)]
            wgs = [(SB(es, "cwg", [128, 16, 512], BF16), S.buf("cwg", i)) for i in range(2)]
            wbs = [(SB(es, "cwb", [128, 8, 512], BF16), S.buf("cwb", i)) for i in range(2)]
            bgb = SB(es, "bgb", [1, 3 * D], BF16); Bbg = S.buf("bgb")
            zacc = SB(es, "zacc", [128, NS, 512]); Bz = S.buf("zacc")
            sgs = [(SB(es, "sg", [128, 512]), S.buf("sg", i)) for i in range(2)]
            zbs = [(SB(es, "zb", [128, 512], BF16), S.buf("zb", i)) for i in range(2)]
            S.dma("pool", bgb[:], b_gate[l:l + 1, :], writes=[Bbg])
            wi = 0; si = 0
            for tiles in chunks(NT, NS):
                load_T(xT, 0, BxT, X, 0, D, tiles, "X", tmpl)
                load_T(xT, 16, BxT, OA, 0, 1024, tiles, "OA", tmpl)
                load_T(xT, 24, BxT, OB, 0, 1024, tiles, "OB", tmpl)
                load_T(xT, 32, BxT, OC, 0, 1024, tiles, "OC", tmpl)
                for cb in range(4):
                    for i in range(3):
                        wg, Bwg = wgs[wi % 2]; wb, Bwb = wbs[wi % 2]; wi += 1
                        gc0 = i * D + cb * 512
                        S.dma("pool", wg[:], w_gate[l, :, gc0:gc0 + 512].rearrange("(k p) n -> p k n", p=128), writes=[Bwg])
                        S.dma("pool", wb[:], w_branch[l, i, :, cb * 512:(cb + 1) * 512].rearrange("(k p) n -> p k n", p=128), writes=[Bwb])
                        for j, t in enumerate(tiles):
                            pg, Bpg = next_ps()
                            for k in range(16):
                                S.op("pe", lambda k=k, pg=pg, wg=wg, j=j: nc.tensor.matmul(pg[:, :], lhsT=xT[:, k, j * 128:(j + 1) * 128], rhs=wg[:, k, :],
                                     start=(k == 0), stop=False), [BxT, Bwg], [Bpg])
                            S.op("pe", lambda pg=pg: nc.tensor.matmul(pg[:, :], lhsT=ones_b[0:1, :], rhs=bgb[0:1, gc0:gc0 + 512], start=False, stop=True), [Bc, Bbg], [Bpg])
                            pu, Bpu = next_ps()
                            for k in range(8):
                                S.op("pe", lambda k=k, pu=pu, wb=wb, j=j: nc.tensor.matmul(pu[:, :], lhsT=xT[:, 16 + i * 8 + k, j * 128:(j + 1) * 128], rhs=wb[:, k, :],
                                     start=(k == 0), stop=(k == 7)), [BxT, Bwb], [Bpu])
                            sg, Bsg = sgs[si % 2]; si += 1
                            S.op("act", lambda pg=pg, sg=sg: nc.scalar.activation(out=sg[:], in_=pg[:, :], func=AF.Sigmoid), [Bpg], [Bsg])
                            if i == 0:
                                S.op("dve", lambda sg=sg, pu=pu, j=j: nc.vector.tensor_tensor(out=zacc[:, j, :], in0=sg[:], in1=pu[:, :], op=ALU.mult), [Bsg, Bpu], [Bz])
                            else:
                                S.op("dve", lambda sg=sg, pu=pu: nc.vector.tensor_tensor(out=sg[:], in0=sg[:], in1=pu[:, :], op=ALU.mult), [Bsg, Bpu], [Bsg])
                                S.op("dve", lambda sg=sg, j=j: nc.vector.tensor_tensor(out=zacc[:, j, :], in0=zacc[:, j, :], in1=sg[:], op=ALU.add), [Bsg, Bz], [Bz])
                    for j, t in enumerate(tiles):
                        zb, Bzb = zbs[j % 2]
                        S.op("act", lambda zb=zb, j=j: nc.scalar.activation(out=zb[:], in_=zacc[:, j, :], func=AF.Copy), [Bz], [Bzb])
                        S.dma("sp", Z[t * 128:(t + 1) * 128, cb * 512:(cb + 1) * 512], zb[:], reads=[Bzb], writes=[S.buf("Z", t)])
            S.barrier()
        with ExitStack() as es:
            zT = SB(es, "zT", [128, 16, NS * 128], BF16); BzT = S.buf("zT")
            tmpl = [(SB(es, "dtA", [128, D], BF16), S.buf("dtA", i)) for i in range(2)]
            wos = [(SB(es, "wo", [128, 16, 512], BF16), S.buf("wo", i)) for i in range(2)]
            rowb = SB(es, "rowb", [128, NS, D]); Brow = S.buf("rowb")
            g_bc = SB(es, "g1", [128, D]); b_bc = SB(es, "b1", [128, D])
            xts = [(SB(es, "x1t", [128, D]), S.buf("x1t", i)) for i in range(2)]
            st = SB(es, "st1", [128, 24]); mv = SB(es, "mv1", [128, 4]); Bst = S.buf("st1")
            S.dma("sp", g_bc[:], IN("ln1_g")[l:l + 1, :].to_broadcast([128, D]), writes=[Bc])
            S.dma("sp", b_bc[:], IN("ln1_b")[l:l + 1, :].to_broadcast([128, D]), writes=[Bc])
            wi = 0
            for tiles in chunks(NT, NS):
                load_T(zT, 0, BzT, Z, 0, D, tiles, "Z", tmpl)
                for cb in range(4):
                    wo, Bwo = wos[wi % 2]; wi += 1
                    S.dma("pool", wo[:], w_out[l, :, cb * 512:(cb + 1) * 512].rearrange("(k p) n -> p k n", p=128), writes=[Bwo])
                    for j, t in enumerate(tiles):
                        p_, Bp = next_ps()
                        for k in range(16):
                            S.op("pe", lambda k=k, p_=p_, wo=wo, j=j: nc.tensor.matmul(p_[:, :], lhsT=zT[:, k, j * 128:(j + 1) * 128], rhs=wo[:, k, :],
                                 start=(k == 0), stop=(k == 15)), [BzT, Bwo], [Bp])
                        evac(rowb[:, j, cb * 512:(cb + 1) * 512], p_[:, :], [Bp], [Brow])
                for j, t in enumerate(tiles):
                    xt, Bxt = xts[j % 2]
                    S.dma("sp", xt[:], X[t * 128:(t + 1) * 128, :], reads=[S.buf("X", t)], writes=[Bxt])
                    S.op("dve", lambda xt=xt, j=j: nc.vector.scalar_tensor_tensor(out=rowb[:, j, :], in0=xt[:], scalar=ALPHA, in1=rowb[:, j, :],
                         op0=ALU.mult, op1=ALU.add), [Bxt, Brow], [Brow])
                    ln_tile(rowb[:, j, :], xt[:], g_bc[:], b_bc[:], st[:], mv[:], Brow, Bxt, Bst, mask_col=mask_of(t))
                    S.dma("sp", X1[t * 128:(t + 1) * 128, :], xt[:], reads=[Bxt], writes=[S.buf("X1", t)])
            S.barrier()

    def phase_ffn(l):
        NS = 5
        moe = (l % 2 == 1)
        nexp = 8 if moe else 1
        with ExitStack() as es:
            x1T = SB(es, "x1T", [128, 16, NS * 128], BF16); Bx1T = S.buf("x1T")
            actT = SB(es, "actT", [128, 44, NS * 128], BF16); Bact = S.buf("actT")
            rowb = SB(es, "frow", [128, NS, D]); Brow = S.buf("frow")
            wgs = [(SB(es, "fwg", [128, 16, 256], BF16), S.buf("fwg", i)) for i in range(2)]
            wus = [(SB(es, "fwu", [128, 16, 256], BF16), S.buf("fwu", i)) for i in range(2)]
            wds = [(SB(es, "fwd", [128, 44, 128], BF16), S.buf("fwd", i)) for i in range(2)]
            sgs = [(SB(es, "fsg", [128, 512]), S.buf("fsg", i)) for i in range(2)]
            gates = SB(es, "gates", [128, NS, 32]); Bga = S.buf("gates")
            if moe:
                wr = SB(es, "wr", [128, 16, 8]); brr = SB(es, "brr", [1, 8]); Bwr = S.buf("wr")
                S.dma("sp", wr[:], IN("moe_w_router").rearrange("(k p) e -> p k e", p=128), writes=[Bwr])
                S.dma("sp", brr[:], IN("moe_b_router")[:, :], writes=[Bwr])
            wi = 0; wdi = 0; si = 0
            for tiles in chunks(NT, NS):
                ntk = len(tiles) * 128
                with ExitStack() as es2:
                    tmpl = [(SB(es2, "etA", [128, D], BF16), S.buf("etA", 0))] * 2
                    load_T(x1T, 0, Bx1T, X1, 0, D, tiles, "X1", tmpl)
                    if moe:
                        xf = SB(es2, "xf", [128, D]); Bxf = S.buf("xf")
                        xfT = SB(es2, "xfT", [128, 16, 128]); BxfT = S.buf("xfT")
                        for j, t in enumerate(tiles):
                            S.dma("sp", xf[:], X1[t * 128:(t + 1) * 128, :], reads=[S.buf("X1", t)], writes=[Bxf])
                            for k0 in range(0, 16, 4):
                                p_, Bp = next_ps()
                                for k in range(4):
                                    S.op("pe", lambda k=k, p_=p_: nc.tensor.transpose(out=p_[:, k * 128:(k + 1) * 128], in_=xf[:, (k0 + k) * 128:(k0 + k + 1) * 128],
                                         identity=ident_f[:]), [Bxf, Bc], [Bp])
                                evac(xfT[:, k0:k0 + 4, :], p_[:, :].rearrange("p (k n) -> p k n", k=4), [Bp], [BxfT])
                            p_, Bp = next_ps()
                            for k in range(16):
                                S.op("pe", lambda k=k, p_=p_: nc.tensor.matmul(p_[:, 0:8], lhsT=xfT[:, k, :], rhs=wr[:, k, :], start=(k == 0), stop=False), [BxfT, Bwr], [Bp])
                            S.op("pe", lambda p_=p_: nc.tensor.matmul(p_[:, 0:8], lhsT=ones_f[0:1, :], rhs=brr[0:1, :], start=False, stop=True), [Bc, Bwr], [Bp])
                            G = gates[:, j, :]
                            S.op("dve", lambda p_=p_, G=G: nc.vector.tensor_copy(out=G[:, 0:8], in_=p_[:, 0:8]), [Bp], [Bga])
                            S.op("dve", lambda G=G: nc.vector.max(out=G[:, 8:16], in_=G[:, 0:8]), [Bga], [Bga])
                            S.op("dve", lambda G=G: nc.vector.tensor_tensor(out=G[:, 24:25], in0=G[:, 8:9], in1=G[:, 9:10], op=ALU.subtract), [Bga], [Bga])
                            S.op("act", lambda G=G: nc.scalar.activation(out=G[:, 25:26], in_=G[:, 24:25], func=AF.Sigmoid), [Bga], [Bga])
                            S.op("dve", lambda G=G: nc.vector.tensor_scalar(out=G[:, 26:27], in0=G[:, 25:26], scalar1=-1.0, scalar2=1.0, op0=ALU.mult, op1=ALU.add), [Bga], [Bga])
                            S.op("dve", lambda G=G: nc.vector.tensor_scalar(out=G[:, 16:24], in0=G[:, 0:8], scalar1=G[:, 8:9], scalar2=G[:, 25:26], op0=ALU.is_equal, op1=ALU.mult), [Bga], [Bga])
                            S.op("dve", lambda G=G: nc.vector.tensor_scalar(out=G[:, 0:8], in0=G[:, 0:8], scalar1=G[:, 9:10], scalar2=G[:, 26:27], op0=ALU.is_equal, op1=ALU.mult), [Bga], [Bga])
                            S.op("dve", lambda G=G: nc.vector.tensor_tensor(out=G[:, 16:24], in0=G[:, 16:24], in1=G[:, 0:8], op=ALU.add), [Bga], [Bga])
                for e in range(nexp):
                    if moe:
                        Wg, Wu, Wd = IN("moe_w_gate")[e], IN("moe_w_up")[e], IN("moe_w_down")[e]
                    else:
                        Wg, Wu, Wd = IN("ffn_w_gate"), IN("ffn_w_up"), IN("ffn_w_down")
                    for f0 in range(0, DFF, 256):
                        wg, Bwg = wgs[wi % 2]; wu, Bwu = wus[wi % 2]; wi += 1
                        S.dma("pool", wg[:], Wg[:, f0:f0 + 256].rearrange("(k p) n -> p k n", p=128), writes=[Bwg])
                        S.dma("pool", wu[:], Wu[:, f0:f0 + 256].rearrange("(k p) n -> p k n", p=128), writes=[Bwu])
                        for fc in range(2):
                            c = f0 // 128 + fc
                            for n0 in range(0, ntk, 512):
                                nw = min(512, ntk - n0)
                                pg, Bpg = next_ps(); pu, Bpu = next_ps()
                                for k in range(16):
                                    S.op("pe", lambda k=k, pg=pg, wg=wg: nc.tensor.matmul(pg[:, 0:nw], lhsT=wg[:, k, fc * 128:(fc + 1) * 128], rhs=x1T[:, k, n0:n0 + nw],
                                         start=(k == 0), stop=(k == 15)), [Bx1T, Bwg], [Bpg])
                                for k in range(16):
                                    S.op("pe", lambda k=k, pu=pu, wu=wu: nc.tensor.matmul(pu[:, 0:nw], lhsT=wu[:, k, fc * 128:(fc + 1) * 128], rhs=x1T[:, k, n0:n0 + nw],
                                         start=(k == 0), stop=(k == 15)), [Bx1T, Bwu], [Bpu])
                                sg, Bsg = sgs[si % 2]; si += 1
                                S.op("act", lambda pg=pg, sg=sg: nc.scalar.activation(out=sg[:, 0:nw], in_=pg[:, 0:nw], func=AF.Silu), [Bpg], [Bsg])
                                S.op("dve", lambda pu=pu, sg=sg, c=c: nc.vector.tensor_tensor(out=actT[:, c, n0:n0 + nw], in0=sg[:, 0:nw], in1=pu[:, 0:nw], op=ALU.mult),
                                     [Bsg, Bpu], [Bact])
                    for c0 in range(0, D, 128):
                        wd, Bwd = wds[wdi % 2]; wdi += 1
                        S.dma("pool", wd[:], Wd[:, c0:c0 + 128].rearrange("(k p) n -> p k n", p=128), writes=[Bwd])
                        for j0 in range(0, len(tiles), 4):
                            jn = min(4, len(tiles) - j0)
                            p_, Bp = next_ps()
                            for jj in range(jn):
                                j = j0 + jj
                                for k in range(44):
                                    S.op("pe", lambda k=k, p_=p_, wd=wd, j=j, jj=jj: nc.tensor.matmul(p_[:, jj * 128:(jj + 1) * 128], lhsT=actT[:, k, j * 128:(j + 1) * 128], rhs=wd[:, k, :],
                                         start=(k == 0), stop=(k == 43)), [Bact, Bwd], [Bp])
                            for jj in range(jn):
                                j = j0 + jj
                                if not moe:
                                    evac(rowb[:, j, c0:c0 + 128], p_[:, jj * 128:(jj + 1) * 128], [Bp], [Brow])
                                elif e == 0:
                                    S.op("dve", lambda p_=p_, j=j, jj=jj: nc.vector.tensor_scalar(out=rowb[:, j, c0:c0 + 128], in0=p_[:, jj * 128:(jj + 1) * 128],
                                         scalar1=gates[:, j, 16 + e:17 + e], scalar2=None, op0=ALU.mult), [Bp, Bga], [Brow])
                                else:
                                    S.op("dve", lambda p_=p_, j=j, jj=jj, e=e: nc.vector.scalar_tensor_tensor(out=rowb[:, j, c0:c0 + 128], in0=p_[:, jj * 128:(jj + 1) * 128],
                                         scalar=gates[:, j, 16 + e:17 + e], in1=rowb[:, j, c0:c0 + 128], op0=ALU.mult, op1=ALU.add), [Bp, Bga, Brow], [Brow])
                with ExitStack() as es2:
                    g_bc = SB(es2, "g2", [128, D]); b_bc = SB(es2, "b2", [128, D])
                    xts = [(SB(es2, "x2t", [128, D]), S.buf("x2t", 0))] * 2
                    st = SB(es2, "st2", [128, 24]); mv = SB(es2, "mv2", [128, 4]); Bst = S.buf("st2")
                    S.dma("sp", g_bc[:], IN("ln2_g")[l:l + 1, :].to_broadcast([128, D]), writes=[Bc])
                    S.dma("sp", b_bc[:], IN("ln2_b")[l:l + 1, :].to_broadcast([128, D]), writes=[Bc])
                    for j, t in enumerate(tiles):
                        xt, Bxt = xts[j % 2]
                        S.dma("sp", xt[:], X1[t * 128:(t + 1) * 128, :], reads=[S.buf("X1", t)], writes=[Bxt])
                        S.op("dve", lambda xt=xt, j=j: nc.vector.scalar_tensor_tensor(out=rowb[:, j, :], in0=xt[:], scalar=ALPHA, in1=rowb[:, j, :],
                             op0=ALU.mult, op1=ALU.add), [Bxt, Brow], [Brow])
                        ln_tile(rowb[:, j, :], xt[:], g_bc[:], b_bc[:], st[:], mv[:], Brow, Bxt, Bst, mask_col=mask_of(t))
                        S.dma("sp", X[t * 128:(t + 1) * 128, :], xt[:], reads=[Bxt], writes=[S.buf("X", t)])
                        if l == 1:
                            S.dma("sp", o_y[t * 128:(t + 1) * 128, :], xt[:], reads=[Bxt], writes=[S.buf("out")])
                    S.barrier()
            S.barrier()

    for l in range(2):
        if on("A"):
            phase_A(l)
        if on("index"):
            phase_index(l)
        if on("attn"):
            phase_attn(l)
        if on("pool"):
            phase_pool(l)
        if on("ret"):
            phase_ret(l)
        if on("merge"):
            phase_merge(l)
        if on("ffn"):
            phase_ffn(l)
        if STOP_AFTER is not None and STOP_AFTER[1] == l:
            break

    S.finish()
    return nc, P


_CACHE = {}


def kernel(**inp):
    f32 = np.float32
    inp = {k: np.asarray(v) for k, v in inp.items()}
    if "nc" not in _CACHE:
        _CACHE["nc"] = build_program()
    nc, P = _CACHE["nc"]
    consts = host_consts()
    xp, xs = inp["x_prompt"], inp["x_sample"]
    in_maps = []
    for c in range(8):
        b = c % 2
        xin = np.zeros((NTOK, D), f32)
        xin[FRONT:FRONT + 16] = inp["meta_tokens"]
        xin[64:64 + 4096] = xp[b]
        xin[NPK:NPK + 64] = xs[2 * c]
        xin[NPK + 64:] = xs[2 * c + 1]
        m = {
            "xin": xin,
            "cache_k": np.ascontiguousarray(inp["cache_k"][:, 2 * c:2 * c + 2].reshape(2, 2, 2048, 1024)),
            "cache_v": np.ascontiguousarray(inp["cache_v"][:, 2 * c:2 * c + 2].reshape(2, 2, 2048, 1024)),
            "cache_ki": np.ascontiguousarray(inp["cache_ki"][:, 2 * c:2 * c + 2]),
            "cache_pool": np.ascontiguousarray(inp["cache_pool"][:, 2 * c:2 * c + 2]),
            "state_ret": np.ascontiguousarray(inp["state_ret"][:, 2 * c:2 * c + 2]),
            "ln_in_g": inp["ln_in_g"].reshape(1, D), "ln_in_b": inp["ln_in_b"].reshape(1, D),
            "w_in": inp["w_in"], "t5_bias": inp["t5_bias"].reshape(1, 256),
            "pool_w": inp["pool_w"], "pool_scale": inp["pool_scale"], "w_branch": inp["w_branch"],
            "w_gate": inp["w_gate"], "b_gate": inp["b_gate"], "w_out": inp["w_out"],
            "ln1_g": inp["ln1_g"], "ln1_b": inp["ln1_b"], "ln2_g": inp["ln2_g"], "ln2_b": inp["ln2_b"],
            "ffn_w_gate": inp["ffn_w_gate"][0], "ffn_w_up": inp["ffn_w_up"][0], "ffn_w_down": inp["ffn_w_down"][0],
            "moe_w_router": inp["moe_w_router"][0], "moe_b_router": inp["moe_b_router"].reshape(1, 8),
            "moe_w_gate": inp["moe_w_gate"][0], "moe_w_up": inp["moe_w_up"][0], "moe_w_down": inp["moe_w_down"][0],
        }
        m.update(consts)
        in_maps.append({k: np.ascontiguousarray(v, dtype=f32) for k, v in m.items() if k in USED_INPUTS})
    if DEV_CORES is not None:
        res = run_bass_kernel_spmd(nc, [in_maps[c] for c in DEV_CORES], core_ids=list(range(len(DEV_CORES))))
        return res.results
    res = run_bass_kernel_spmd(nc, in_maps, core_ids=list(range(8)))
    R = res.results
    T = 4112
    y_prompt = np.stack([R[b]["o_y"][64:64 + 4096] for b in range(2)])
    y_sample = np.concatenate([R[c]["o_y"][NPK:].reshape(2, 64, D) for c in range(8)])

    def pr(name, w):
        return np.stack([np.stack([R[b][name][l][FRONT:FRONT + T] for b in range(2)]) for l in range(2)])

    def sm(name, w):
        return np.stack([np.concatenate([R[c][name][l][NPK:].reshape(2, 64, w) for c in range(8)]) for l in range(2)])

    k_p = pr("o_k", 1024).reshape(2, 2, T, 8, 128); v_p = pr("o_v", 1024).reshape(2, 2, T, 8, 128); ki_p = pr("o_ki", 64)
    pool_p = np.stack([np.stack([R[b]["o_u"][l][FRONT + T - 15:FRONT + T] for b in range(2)]) for l in range(2)])
    ret_p = np.stack([np.stack([R[b]["o_retp"][l] for b in range(2)]) for l in range(2)])
    k_s = sm("o_k", 1024).reshape(2, 16, 64, 8, 128); v_s = sm("o_v", 1024).reshape(2, 16, 64, 8, 128); ki_s = sm("o_ki", 64)
    pool_s = sm("o_u", 1024)[:, :, 49:64]
    ret_s = np.stack([np.concatenate([R[c]["o_rets"][l] for c in range(8)]) for l in range(2)])
    return (y_prompt.astype(f32), y_sample.astype(f32), k_p, v_p, ki_p, pool_p, ret_p, k_s, v_s, ki_s, pool_s, ret_s)
```

```python
import numpy as np
import ml_dtypes
import concourse.bass as bass
import concourse.mybir as mybir
from concourse.bass_utils import run_bass_kernel_spmd

F32 = mybir.dt.float32
BF16 = mybir.dt.bfloat16
ALU = mybir.AluOpType
AF = mybir.ActivationFunctionType

D = 2048
NTP = 33
NT = 34
NTOK = NT * 128
NPK = NTP * 128
FRONT = 48
INW = 9296
DFF = 5632
BIG = 30000.0
ALPHA = 4.0 ** 0.25
EPS = 1e-5
C_QA, C_KA, C_VA, C_QI, C_KI, C_WI, C_UB, C_QC, C_KC, C_VC, C_GC = 0, 1024, 2048, 3072, 4096, 4160, 4176, 5200, 6224, 7248, 8272
GAM = [1.0 - 2.0 ** (-5.0 - h) for h in range(8)]
STOP_AFTER = None
USED_INPUTS = set()
DEV_CORES = None
ATT_MODE = 2


class Buf:
    __slots__ = ("w", "r")

    def __init__(self):
        self.w = None
        self.r = []


class Sync:
    EPOCH = 10 ** 9
    NSLOT = 40

    def __init__(self, nc):
        self.nc = nc
        self.eng = {"pe": nc.tensor, "act": nc.scalar, "dve": nc.vector, "pool": nc.gpsimd, "sp": nc.sync}
        self.sem = {}
        self.ep = {}
        self.cnt = {}
        for e in self.eng:
            self.ep[e] = 0
            self.cnt[e] = 0
            self.sem[(e, 0)] = nc.alloc_semaphore(name=f"s_{e}_0")
        self.dsem = [nc.alloc_semaphore(name=f"d_{i}") for i in range(self.NSLOT)]
        self.dcnt = [0] * self.NSLOT
        self.dnext = 0
        self.waited = {e: {} for e in self.eng}
        self.bufs = {}

    def buf(self, *key):
        b = self.bufs.get(key)
        if b is None:
            b = self.bufs[key] = Buf()
        return b

    def _wait(self, e, ev):
        if ev is None:
            return
        key, val = ev
        if key[0] == e and e == "pe":
            return
        if key[0] != "dma" and key[0] == e and key[1] != self.ep[e]:
            return
        if self.waited[e].get(key, 0) >= val:
            return
        sem = self.dsem[key[1]] if key[0] == "dma" else self.sem[key]
        self.eng[e].wait_ge(sem, val)
        self.waited[e][key] = val

    def _deps(self, e, reads, writes):
        for b in reads:
            self._wait(e, b.w)
        for b in writes:
            self._wait(e, b.w)
            for ev in b.r:
                self._wait(e, ev)

    def _commit(self, ev, reads, writes):
        for b in reads:
            b.r.append(ev)
            if len(b.r) > 64:
                b.r = b.r[-64:]
        for b in writes:
            b.w = ev
            b.r = []

    def op(self, e, fn, reads=(), writes=()):
        self._deps(e, reads, writes)
        if self.cnt[e] >= self.EPOCH:
            self.ep[e] += 1
            self.cnt[e] = 0
            self.sem[(e, self.ep[e])] = self.nc.alloc_semaphore(name=f"s_{e}_{self.ep[e]}")
        ins = fn()
        self.cnt[e] += 1
        ins.then_inc(self.sem[(e, self.ep[e])], 1)
        ev = ((e, self.ep[e]), self.cnt[e])
        self._commit(ev, reads, writes)
        return ev

    def dma(self, q, out, in_, reads=(), writes=()):
        self._deps(q, reads, writes)
        s = self.dnext
        self.dnext = (s + 1) % self.NSLOT
        if self.dcnt[s]:
            self._wait(q, (("dma", s), 16 * self.dcnt[s]))
        ins = self.eng[q].dma_start(out=out, in_=in_)
        self.dcnt[s] += 1
        ins.then_inc(self.dsem[s], 16)
        ev = (("dma", s), 16 * self.dcnt[s])
        self._commit(ev, reads, writes)
        return ev

    def barrier(self):
        evs = []
        for e in self.eng:
            if self.cnt[e]:
                evs.append(((e, self.ep[e]), self.cnt[e]))
        for s in range(self.NSLOT):
            if self.dcnt[s]:
                evs.append((("dma", s), 16 * self.dcnt[s]))
        for e in self.eng:
            for ev in evs:
                self._wait(e, ev)

    def finish(self):
        self.barrier()


def _bucket(rel):
    n = np.abs(rel)
    large = 8 + (np.log(np.maximum(n, 1).astype(np.float32) / 8) / np.log(16.0) * 8).astype(np.int32)
    large = np.minimum(large, 15)
    return np.where(rel > 0, 16, 0) + np.where(n < 8, n, large)


def host_consts():
    c = {}
    q = np.arange(128)[:, None]
    j = np.arange(256)[None, :]
    bk = _bucket(j - 128 - q)
    oh = np.zeros((128, 32, 256), np.float32)
    for b in range(32):
        oh[:, b, :] = (bk == b)
    oh[:, 15, :] -= 1.0
    c["c_oh"] = oh.reshape(128, 32 * 256)
    tm = np.ones((NT, 128), np.float32)
    tm[0, :FRONT] = 0
    tm[32, 64:] = 0
    c["c_tmask"] = np.ascontiguousarray(tm.T)
    c["c_ident"] = np.eye(128, dtype=np.float32)
    wins = (2, 4, 8, 16)
    cur = np.zeros((4, 128, 128), np.float32)
    prv = np.zeros((4, 128, 128), np.float32)
    cur0 = np.zeros((4, 128, 128), np.float32)
    sprv = np.zeros((4, 16, 64), np.float32)
    for g, w in enumerate(wins):
        for t in range(128):
            for i in range(w):
                tp = t - i
                if tp >= 0:
                    cur[g, tp, t] += 1.0 / w
                else:
                    prv[g, 128 + tp, t] += 1.0 / w
            cur[g, t, t] -= 1.0
            n = min(w, t - FRONT + 1) if t >= FRONT else 1
            for i in range(w):
                tp = t - i
                if tp >= 0:
                    cur0[g, tp, t] += 1.0 / n
            cur0[g, t, t] -= 1.0
        for t in range(64):
            for i in range(w):
                tp = t - i
                if tp < 0:
                    sprv[g, 15 + tp, t] += 1.0 / w
    c["c_pcur"] = np.ascontiguousarray(cur.transpose(1, 0, 2)).reshape(128, 512)
    c["c_pprv"] = np.ascontiguousarray(prv.transpose(1, 0, 2)).reshape(128, 512)
    c["c_pcur0"] = np.ascontiguousarray(cur0.transpose(1, 0, 2)).reshape(128, 512)
    c["c_psprv"] = np.ascontiguousarray(sprv.transpose(1, 0, 2)).reshape(16, 256)
    pos = np.zeros(NTOK, np.float64)
    pos[:NPK] = np.arange(NPK) - 64
    pos[NPK:NPK + 64] = 2048 + np.arange(64)
    pos[NPK + 64:] = 2048 + np.arange(64)
    inv = 10000.0 ** (-np.arange(64, dtype=np.float64) / 64)
    ang = (pos[:, None].astype(np.float32) * inv[None, :].astype(np.float32)).astype(np.float32)
    c["c_cos"] = np.cos(ang).astype(np.float32)
    c["c_sin"] = np.sin(ang).astype(np.float32)
    nloc = np.arange(NTOK) % 128
    nloc[NPK:] = np.arange(128) % 64
    g = np.array(GAM, np.float64)
    c["c_qdec"] = (g[None, :] ** (nloc[:, None] + 1)).astype(np.float32)
    c["c_kdec"] = ((128 ** -0.5) * g[None, :] ** (-(nloc[:, None] + 1.0))).astype(np.float32)
    tri = (np.arange(128)[:, None] <= np.arange(128)[None, :]).astype(np.float32)
    c["c_tri"] = tri
    return c


IN_SHAPES = {
    "xin": [NTOK, D], "cache_k": [2, 2, 2048, 1024], "cache_v": [2, 2, 2048, 1024], "cache_ki": [2, 2, 2048, 64],
    "cache_pool": [2, 2, 15, 1024], "state_ret": [2, 2, 8, 128, 128], "ln_in_g": [1, D], "ln_in_b": [1, D],
    "w_in": [2, D, INW], "t5_bias": [1, 256], "pool_w": [2, 4, 256, 256], "pool_scale": [2, 1024],
    "w_branch": [2, 3, 1024, D], "w_gate": [2, D, 3 * D], "b_gate": [2, 3 * D], "w_out": [2, D, D],
    "ln1_g": [2, D], "ln1_b": [2, D], "ln2_g": [2, D], "ln2_b": [2, D],
    "ffn_w_gate": [D, DFF], "ffn_w_up": [D, DFF], "ffn_w_down": [DFF, D],
    "moe_w_router": [D, 8], "moe_b_router": [1, 8], "moe_w_gate": [8, D, DFF], "moe_w_up": [8, D, DFF], "moe_w_down": [8, DFF, D],
    "c_oh": [128, 32 * 256], "c_tmask": [128, NT], "c_ident": [128, 128], "c_pcur": [128, 512], "c_pprv": [128, 512],
    "c_pcur0": [128, 512], "c_psprv": [16, 256], "c_cos": [NTOK, 64], "c_sin": [NTOK, 64], "c_qdec": [NTOK, 8],
    "c_kdec": [NTOK, 8], "c_tri": [128, 128],
}
DEBUG_SCRATCH = False
STAGES = None


def build_program():
    from contextlib import ExitStack
    nc = bass.Bass("TRN2", target_bir_lowering=False)
    S = Sync(nc)
    P = {}
    uid = [0]

    def IN(name):
        if name not in P:
            P[name] = nc.dram_tensor(name, list(IN_SHAPES[name]), F32, kind="ExternalInput").ap()
            USED_INPUTS.add(name)
        return P[name]

    def dout(name, shape, dt=F32):
        P[name] = nc.dram_tensor(name, list(shape), dt, kind="ExternalOutput").ap()
        return P[name]

    def dscr(name, shape, dt=F32):
        if DEBUG_SCRATCH:
            return dout(name, shape, dt)
        return nc.dram_tensor(name, list(shape), dt, kind="Internal").ap()

    def SB(es, name, shape, dt=F32):
        uid[0] += 1
        return es.enter_context(nc.sbuf_tensor(f"{name}_{uid[0]}", list(shape), dt))

    def on(stage):
        return STAGES is None or stage in STAGES

    xin = IN("xin")
    o_y = dout("o_y", [NTOK, D])
    o_k = dout("o_k", [2, NTOK, 1024]); o_v = dout("o_v", [2, NTOK, 1024]); o_ki = dout("o_ki", [2, NTOK, 64])
    o_u = dout("o_u", [2, NTOK, 1024])
    o_retp = dout("o_retp", [2, 8, 128, 128]); o_rets = dout("o_rets", [2, 2, 8, 128, 128])

    X = dscr("X", [NTOK, D]); X1 = dscr("X1", [NTOK, D])
    H = dscr("H", [NTOK, INW])
    Mm = dscr("Mm", [NTOK, NPK], BF16)
    OA = dscr("OA", [NTOK, 1024], BF16); OB = dscr("OB", [NTOK, 1024], BF16); OC = dscr("OC", [NTOK, 1024], BF16)
    Z = dscr("Z", [NTOK, D], BF16)

    ps = [nc.alloc_psum_tensor(f"ps{i}", [128, 512], F32) for i in range(8)]
    psb = [S.buf("ps", i) for i in range(8)]
    psi = [0]

    def next_ps():
        i = psi[0]
        psi[0] = (i + 1) % 8
        return ps[i], psb[i]

    acci = [0]

    def next_acc_ps():
        acci[0] ^= 1
        return ps[6 + acci[0]], psb[6 + acci[0]]

    ident_b = nc.alloc_sbuf_tensor("ident_b", [128, 128], BF16)
    ident_f = nc.alloc_sbuf_tensor("ident_f", [128, 128], F32)
    tmask = nc.alloc_sbuf_tensor("tmask", [128, NT], F32)
    ones_b = nc.alloc_sbuf_tensor("ones_b", [1, 128], BF16)
    ones_f = nc.alloc_sbuf_tensor("ones_f", [1, 128], F32)
    Bc = S.buf("consts")
    S.dma("pool", ident_b[:], IN("c_ident")[:, :], writes=[Bc])
    S.dma("sp", ident_f[:], IN("c_ident")[:, :], writes=[Bc])
    S.dma("sp", tmask[:], IN("c_tmask")[:, :], writes=[Bc])
    S.op("dve", lambda: nc.vector.memset(ones_b[:], 1.0), [], [Bc])
    S.op("dve", lambda: nc.vector.memset(ones_f[:], 1.0), [], [Bc])
    evac_rr = [0]

    def evac(out, in_, reads, writes):
        evac_rr[0] ^= 1
        if evac_rr[0]:
            return S.op("act", lambda: nc.scalar.activation(out=out, in_=in_, func=AF.Copy), reads, writes)
        return S.op("dve", lambda: nc.vector.tensor_copy(out=out, in_=in_), reads, writes)

    def rstd_from_var(var_ap, tmp_ap, out_ap, Bst):
        S.op("dve", lambda: nc.vector.tensor_scalar(out=tmp_ap, in0=var_ap, scalar1=EPS, scalar2=None, op0=ALU.add), [Bst], [Bst])
        S.op("act", lambda: nc.scalar.activation(out=tmp_ap, in_=tmp_ap, func=AF.Sqrt), [Bst], [Bst])
        S.op("dve", lambda: nc.vector.reciprocal(out=out_ap, in_=tmp_ap), [Bst], [Bst])

    def ln_tile(src, dst, g_bc, b_bc, stats, mv, Bsrc, Bdst, Bst, mask_col=None):
        for c in range(4):
            S.op("dve", lambda c=c: nc.vector.bn_stats(out=stats[:, c * 6:(c + 1) * 6], in_=src[:, c * 512:(c + 1) * 512]), [Bsrc], [Bst])
        S.op("dve", lambda: nc.vector.bn_aggr(out=mv[:, 0:2], in_=stats[:, 0:24]), [Bst], [Bst])
        rstd_from_var(mv[:, 1:2], mv[:, 3:4], mv[:, 2:3], Bst)
        if mask_col is not None:
            S.op("dve", lambda: nc.vector.tensor_tensor(out=mv[:, 2:3], in0=mv[:, 2:3], in1=mask_col, op=ALU.mult), [Bst, Bc], [Bst])
        S.op("dve", lambda: nc.vector.tensor_scalar(out=dst, in0=src, scalar1=mv[:, 0:1], scalar2=mv[:, 2:3],
                                                     op0=ALU.subtract, op1=ALU.mult), [Bsrc, Bst], [Bdst])
        S.op("dve", lambda: nc.vector.tensor_tensor(out=dst, in0=dst, in1=g_bc, op=ALU.mult), [Bdst, Bc], [Bdst])
        if mask_col is not None:
            S.op("dve", lambda: nc.vector.scalar_tensor_tensor(out=dst, in0=b_bc, scalar=mask_col, in1=dst,
                                                                op0=ALU.mult, op1=ALU.add), [Bdst, Bc], [Bdst])
        else:
            S.op("dve", lambda: nc.vector.tensor_tensor(out=dst, in0=dst, in1=b_bc, op=ALU.add), [Bdst, Bc], [Bdst])

    def mask_of(t):
        return tmask[:, t:t + 1] if t in (0, 32) else None

    def load_T(dst, kofs, Bdst, src, c0, K, tiles, srcbuf, tmpl):
        KC = K // 128
        for j, t in enumerate(tiles):
            tm_, Bt = tmpl[j % 2]
            S.dma("pool", tm_[:, 0:K], src[t * 128:(t + 1) * 128, c0:c0 + K], reads=[S.buf(srcbuf, t)], writes=[Bt])
            for k0 in range(0, KC, 4):
                kn = min(4, KC - k0)
                p_, Bp = next_ps()
                pv = p_[:].bitcast(BF16)
                for k in range(kn):
                    S.op("pe", lambda k=k, pv=pv, tm_=tm_: nc.tensor.transpose(out=pv[:, k * 128:(k + 1) * 128],
                         in_=tm_[:, (k0 + k) * 128:(k0 + k + 1) * 128], identity=ident_b[:]), [Bt, Bc], [Bp])
                evac(dst[:, kofs + k0:kofs + k0 + kn, j * 128:(j + 1) * 128],
                     pv[:, 0:kn * 128].rearrange("p (k n) -> p k n", k=kn), [Bp], [Bdst])

    def chunks(n, c):
        return [list(range(s, min(s + c, n))) for s in range(0, n, c)]

    with ExitStack() as es:
        g_bc = SB(es, "g_bc", [128, D]); b_bc = SB(es, "b_bc", [128, D])
        S.dma("sp", g_bc[:], IN("ln_in_g")[0:1, :].to_broadcast([128, D]), writes=[Bc])
        S.dma("sp", b_bc[:], IN("ln_in_b")[0:1, :].to_broadcast([128, D]), writes=[Bc])
        xt = [SB(es, "xt", [128, D]) for i in range(2)]
        xo = [SB(es, "xo", [128, D]) for i in range(2)]
        st = [SB(es, "st", [128, 24]) for i in range(2)]
        mv = [SB(es, "mv", [128, 4]) for i in range(2)]
        for t in range(NT):
            i = t % 2
            Bx, Bo, Bs_ = S.buf("xt", i), S.buf("xo", i), S.buf("st", i)
            S.dma("sp", xt[i][:], xin[t * 128:(t + 1) * 128, :], writes=[Bx])
            ln_tile(xt[i][:], xo[i][:], g_bc[:], b_bc[:], st[i][:], mv[i][:], Bx, Bo, Bs_, mask_col=tmask[:, t:t + 1])
            S.dma("sp", X[t * 128:(t + 1) * 128, :], xo[i][:], reads=[Bo], writes=[S.buf("X", t)])
        S.barrier()

    def phase_A(l):
        with ExitStack() as es:
            NS = 17
            xT = SB(es, "xT", [128, 16, NS * 128], BF16)
            tmpl = [(SB(es, "tA", [128, D], BF16), S.buf("tA", i)) for i in range(2)]
            wbs = [(SB(es, "wb", [128, 16, 512], BF16), S.buf("wb", i)) for i in range(2)]
            hss = [(SB(es, "hs", [128, 512], F32), S.buf("hs", i)) for i in range(3)]
            BxT = S.buf("xT")
            wi = 0; hi = 0
            w_in = IN("w_in")
            for tiles in chunks(NT, NS):
                load_T(xT, 0, BxT, X, 0, D, tiles, "X", tmpl)
                for c0 in range(0, INW, 512):
                    cw = min(512, INW - c0)
                    wb, Bw = wbs[wi % 2]; wi += 1
                    S.dma("pool", wb[:, :, 0:cw], w_in[l, :, c0:c0 + cw].rearrange("(k p) n -> p k n", p=128), writes=[Bw])
                    for j, t in enumerate(tiles):
                        p_, Bp = next_ps()
                        for k in range(16):
                            S.op("pe", lambda k=k, p_=p_, wb=wb, j=j: nc.tensor.matmul(p_[:, 0:cw], lhsT=xT[:, k, j * 128:(j + 1) * 128],
                                 rhs=wb[:, k, 0:cw], start=(k == 0), stop=(k == 15)), [BxT, Bw], [Bp])
                        hs, Bh = hss[hi % 3]; hi += 1
                        evac(hs[:, 0:cw], p_[:, 0:cw], [Bp], [Bh])
                        S.dma("sp", H[t * 128:(t + 1) * 128, c0:c0 + cw], hs[:, 0:cw], reads=[Bh], writes=[S.buf("H", t)])
            S.barrier()
        allH = [S.buf("H", t) for t in range(NT)]
        S.dma("sp", o_k[l, :, :], H[:, C_KA:C_KA + 1024], reads=allH, writes=[S.buf("out")])
        S.dma("sp", o_v[l, :, :], H[:, C_VA:C_VA + 1024], reads=allH, writes=[S.buf("out")])
        S.dma("sp", o_ki[l, :, :], H[:, C_KI:C_KI + 64], reads=allH, writes=[S.buf("out")])
        S.dma("sp", o_u[l, :, :], H[:, C_UB:C_UB + 1024], reads=allH, writes=[S.buf("out")])
        S.barrier()

    def transpose_into(dst_ap, src_ap, n, m, Bsrc, Bdst, f32=False):
        p_, Bp = next_ps()
        pv = p_[:] if f32 else p_[:].bitcast(BF16)
        idt = ident_f if f32 else ident_b
        S.op("pe", lambda: nc.tensor.transpose(out=pv[0:m, 0:n], in_=src_ap, identity=idt[0:n, 0:n]), [Bsrc, Bc], [Bp])
        evac(dst_ap, pv[0:m, 0:n], [Bp], [Bdst])

    def phase_index(l):
        with ExitStack() as es:
            kIT = SB(es, "kIT", [64, NPK], F32); BkIT = S.buf("kIT")
            kITs = SB(es, "kITs", [64, 2112], F32); BkITs = S.buf("kITs")
            acc = SB(es, "acc", [128, NPK]); Bacc = S.buf("acc")
            wk = SB(es, "wk", [128, NPK]); Bwk = S.buf("wk")
            mts = [(SB(es, "mt", [128, NPK], BF16), S.buf("mt", i)) for i in range(2)]
            qis = [(SB(es, "qi", [128, 1024]), S.buf("qi", i)) for i in range(2)]
            wis = [(SB(es, "wi", [128, 48]), S.buf("wi", i)) for i in range(2)]
            qss = [(SB(es, "qs", [128, 1024], F32), S.buf("qs", i)) for i in range(2)]
            qsTs = [(SB(es, "qsT", [64, 16, 128], F32), S.buf("qsT", i)) for i in range(2)]
            rls = [(SB(es, "rl", [128, 512]), S.buf("rl", i)) for i in range(3)]
            m8 = SB(es, "m8", [128, 16]); Bm8 = S.buf("m8")
            kits = [(SB(es, "kit", [128, 64], F32), S.buf("kit", i)) for i in range(2)]
            cache_ki = IN("cache_ki")
            for t in range(NTP):
                kt_, Bk = kits[t % 2]
                S.dma("sp", kt_[:], H[t * 128:(t + 1) * 128, C_KI:C_KI + 64], reads=[S.buf("H", t)], writes=[Bk])
                transpose_into(kIT[:, t * 128:(t + 1) * 128], kt_[:], 128, 64, Bk, BkIT, f32=True)
            rli = [0]

            def unit(ui, row0, n, kT, BkT, nk, prompt):
                qi, Bqi = qis[ui % 2]; wi_, Bwi = wis[ui % 2]; qs, Bqs = qss[ui % 2]; qsT, BqsT = qsTs[ui % 2]; mt, Bmt = mts[ui % 2]
                tb = S.buf("H", row0 // 128)
                S.dma("sp", qi[0:n, :], H[row0:row0 + n, C_QI:C_QI + 1024], reads=[tb], writes=[Bqi])
                S.dma("sp", wi_[0:n, 0:16], H[row0:row0 + n, C_WI:C_WI + 16], reads=[tb], writes=[Bwi])
                S.op("act", lambda: nc.scalar.activation(out=wi_[0:n, 16:32], in_=wi_[0:n, 0:16], func=AF.Abs), [Bwi], [Bwi])
                S.op("dve", lambda: nc.vector.tensor_scalar(out=wi_[0:n, 32:48], in0=wi_[0:n, 0:16], scalar1=0.0, scalar2=2.0, op0=ALU.is_gt, op1=ALU.mult), [Bwi], [Bwi])
                S.op("dve", lambda: nc.vector.tensor_scalar(out=wi_[0:n, 32:48], in0=wi_[0:n, 32:48], scalar1=-1.0, scalar2=None, op0=ALU.add), [Bwi], [Bwi])
                S.op("dve", lambda: nc.vector.tensor_tensor(out=qs[0:n, :].rearrange("p (h d) -> p h d", h=16), in0=qi[0:n, :].rearrange("p (h d) -> p h d", h=16),
                     in1=wi_[0:n, 16:32].unsqueeze(2).to_broadcast([n, 16, 64]), op=ALU.mult), [Bqi, Bwi], [Bqs])
                for h0 in range(0, 16, 4):
                    p_, Bp = next_ps()
                    pv = p_[:]
                    for k in range(4):
                        S.op("pe", lambda k=k, pv=pv: nc.tensor.transpose(out=pv[0:64, k * 128:k * 128 + n], in_=qs[0:n, (h0 + k) * 64:(h0 + k + 1) * 64],
                             identity=ident_f[0:n, 0:n]), [Bqs, Bc], [Bp])
                    evac(qsT[:, h0:h0 + 4, 0:n], pv[0:64, 0:512].rearrange("p (k n) -> p k n", k=4)[:, :, 0:n], [Bp], [BqsT])
                for kb in range(0, nk, 512):
                    w = min(512, nk - kb)
                    for h in range(16):
                        p_, Bp = next_ps()
                        S.op("pe", lambda h=h, p_=p_: nc.tensor.matmul(p_[0:n, 0:w], lhsT=qsT[:, h, 0:n], rhs=kT[:, kb:kb + w], start=True, stop=True),
                             [BqsT, BkT], [Bp])
                        rl, Brl = rls[rli[0] % 3]; rli[0] += 1
                        S.op("act", lambda p_=p_, rl=rl: nc.scalar.activation(out=rl[0:n, 0:w], in_=p_[0:n, 0:w], func=AF.Relu), [Bp], [Brl])
                        if h == 0:
                            S.op("dve", lambda rl=rl: nc.vector.tensor_scalar(out=acc[0:n, kb:kb + w], in0=rl[0:n, 0:w], scalar1=wi_[0:n, 32:33], scalar2=None,
                                 op0=ALU.mult), [Brl, Bwi], [Bacc])
                        else:
                            S.op("dve", lambda rl=rl, h=h: nc.vector.scalar_tensor_tensor(out=acc[0:n, kb:kb + w], in0=rl[0:n, 0:w], scalar=wi_[0:n, 32 + h:33 + h],
                                 in1=acc[0:n, kb:kb + w], op0=ALU.mult, op1=ALU.add), [Brl, Bwi, Bacc], [Bacc])
                if prompt:
                    S.op("dve", lambda: nc.vector.memset(acc[0:n, 0:FRONT], -BIG), [], [Bacc])
                    S.op("dve", lambda: nc.vector.memset(acc[0:64, nk - 64:nk], -BIG), [], [Bacc])
                for c0 in range(0, nk, 2048):
                    c1 = min(nk, c0 + 2048)
                    S.op("act", lambda c0=c0, c1=c1: nc.scalar.activation(out=wk[0:n, c0:c1], in_=acc[0:n, c0:c1], func=AF.Copy), [Bacc], [Bwk])
                for r in range(min(32, nk // 8)):
                    S.op("dve", lambda: nc.vector.max(out=m8[0:n, 0:8], in_=wk[0:n, 0:nk]), [Bwk], [Bm8])
                    if r < min(32, nk // 8) - 1:
                        S.op("dve", lambda: nc.vector.match_replace(out=wk[0:n, 0:nk], in_to_replace=m8[0:n, 0:8], in_values=wk[0:n, 0:nk], imm_value=-BIG),
                             [Bwk, Bm8], [Bwk])
                S.op("dve", lambda: nc.vector.tensor_scalar(out=m8[0:n, 8:9], in0=m8[0:n, 7:8], scalar1=-BIG / 2, scalar2=None, op0=ALU.max), [Bm8], [Bm8])
                for c0 in range(0, nk, 2048):
                    c1 = min(nk, c0 + 2048)
                    S.op("dve", lambda c0=c0, c1=c1: nc.vector.tensor_scalar(out=mt[0:n, c0:c1], in0=acc[0:n, c0:c1], scalar1=m8[0:n, 8:9], scalar2=-BIG, op0=ALU.is_lt, op1=ALU.mult),
                         [Bacc, Bm8], [Bmt])
                S.dma("sp", Mm[row0:row0 + n, 0:nk], mt[0:n, 0:nk], reads=[Bmt], writes=[S.buf("Mm", row0 // 64)])

            for t in range(NTP):
                unit(t, t * 128, 128, kIT, BkIT, 128 * (t + 1), True)
            for s in range(2):
                for t in range(16):
                    kt_, Bk = kits[t % 2]
                    S.dma("sp", kt_[:], cache_ki[l, s, t * 128:(t + 1) * 128, :], writes=[Bk])
                    transpose_into(kITs[:, t * 128:(t + 1) * 128], kt_[:], 128, 64, Bk, BkITs, f32=True)
                kt_, Bk = kits[0]
                r0 = NPK + 64 * s
                S.dma("sp", kt_[0:64, :], H[r0:r0 + 64, C_KI:C_KI + 64], reads=[S.buf("H", 33)], writes=[Bk])
                transpose_into(kITs[:, 2048:2112], kt_[0:64, :], 64, 64, Bk, BkITs, f32=True)
                unit(NTP + s, r0, 64, kITs, BkITs, 2112, False)
            S.barrier()

    def phase_attn(l):
        scale = 128 ** -0.5
        with ExitStack() as es:
            NB = SB(es, "NB", [128, 8, 256]); BNB = S.buf("NB")
            TB = SB(es, "TB", [128, 256])
            ohb = [SB(es, "ohb", [128, 256]) for _ in range(2)]
            S.dma("sp", TB[:], IN("t5_bias")[0:1, :].to_broadcast([128, 256]), writes=[S.buf("TB")])
            S.op("dve", lambda: nc.vector.memset(NB[:], 0.0), [], [BNB])
            for b in range(32):
                o_, Bo = ohb[b % 2], S.buf("ohb", b % 2)
                S.dma("sp", o_[:], IN("c_oh")[:, b * 256:(b + 1) * 256], writes=[Bo])
                for h in range(8):
                    S.op("dve", lambda h=h, o_=o_, b=b: nc.vector.scalar_tensor_tensor(out=NB[:, h, :], in0=o_[:], scalar=TB[:, b * 8 + h:b * 8 + h + 1],
                         in1=NB[:, h, :], op0=ALU.mult, op1=ALU.add), [Bo, S.buf("TB"), BNB], [BNB])
            kT = SB(es, "kT", [128, NPK], BF16); BkT = S.buf("kT")
            qT = SB(es, "qT", [128, NTOK], BF16); BqT = S.buf("qT")
            V = SB(es, "V", [128, 33, 132], BF16); BV = S.buf("V")
            kTs = SB(es, "kTs", [128, 2112], BF16); BkTs = S.buf("kTs")
            Vs = SB(es, "Vs", [128, 17, 132], BF16); BVs = S.buf("Vs")
            tqs = [(SB(es, "tq", [128, 128], BF16), S.buf("tq", i)) for i in range(3)]
            mts = [(SB(es, "amt", [128, NPK], BF16), S.buf("amt", i)) for i in range(2)]
            Ls = [(SB(es, "L", [128, NPK]), S.buf("L", i)) for i in range(2)]
            Ps = [(SB(es, "P", [128, NPK], BF16), S.buf("P", i)) for i in range(2)]
            PTs = [(SB(es, "PT", [128, 8, 128], BF16), S.buf("PT", i)) for i in range(3)]
            obs = [(SB(es, "ob", [128, 128], BF16), S.buf("ob", i)) for i in range(2)]
            rvs = [(SB(es, "rv", [128, 2]), S.buf("rv", i)) for i in range(2)]
            cache_k = IN("cache_k"); cache_v = IN("cache_v")
            S.op("dve", lambda: nc.vector.memset(V[:, :, 128:132], 1.0), [], [BV])
            S.op("dve", lambda: nc.vector.memset(Vs[:, :, 128:132], 1.0), [], [BVs])
            cnt = [0]

            def attend(n, q_ap, kT_, BkT_, V_, BV_, nk, mrow0, nb_lo, nb_ap, orow0, h):
                u = cnt[0]; cnt[0] += 1
                mt, Bmt = mts[u % 2]; L, BL = Ls[u % 2]; Pm, BP = Ps[u % 2]; ob, Bob = obs[u % 2]; rv, Brv = rvs[u % 2]
                S.dma("sp", mt[0:n, 0:nk], Mm[mrow0:mrow0 + n, 0:nk], reads=[S.buf("Mm", mrow0 // 64)], writes=[Bmt])
                for kb in range(0, nk, 512):
                    w = min(512, nk - kb)
                    p_, Bp = next_ps()
                    S.op("pe", lambda p_=p_: nc.tensor.matmul(p_[0:n, 0:w], lhsT=q_ap, rhs=kT_[:, kb:kb + w], start=True, stop=True), [BqT, BkT_], [Bp])
                    S.op("dve", lambda p_=p_: nc.vector.scalar_tensor_tensor(out=L[0:n, kb:kb + w], in0=p_[0:n, 0:w], scalar=scale, in1=mt[0:n, kb:kb + w],
                         op0=ALU.mult, op1=ALU.add), [Bp, Bmt], [BL])
                nbw = nb_ap.shape[-1]
                S.op("dve", lambda: nc.vector.tensor_tensor(out=L[0:n, nb_lo:nb_lo + nbw], in0=L[0:n, nb_lo:nb_lo + nbw], in1=nb_ap, op=ALU.add), [BL, BNB], [BL])
                if ATT_MODE == 0:
                    return
                for c0 in range(0, nk, 2048):
                    c1 = min(nk, c0 + 2048)
                    S.op("act", lambda c0=c0, c1=c1: nc.scalar.activation(out=Pm[0:n, c0:c1], in_=L[0:n, c0:c1], func=AF.Exp), [BL], [BP])
                if ATT_MODE == 1:
                    return
                po, Bpo = next_ps()
                nkt = (nk + 127) // 128
                for k0 in range(0, nkt, 8):
                    kn = min(8, nkt - k0)
                    p_, Bp = next_ps()
                    pv = p_[:].bitcast(BF16)
                    kws = []
                    for k in range(kn):
                        kw = min(128, nk - (k0 + k) * 128)
                        kws.append(kw)
                        S.op("pe", lambda k=k, kw=kw, pv=pv: nc.tensor.transpose(out=pv[0:kw, k * 128:k * 128 + n], in_=Pm[0:n, (k0 + k) * 128:(k0 + k) * 128 + kw],
                             identity=ident_b[0:n, 0:n]), [BP, Bc], [Bp])
                    PT, BPT = PTs[(u + k0 // 8) % 3]
                    if min(kws) == 128:
                        evac(PT[:, 0:kn, 0:n], pv[:, 0:1024].rearrange("p (k n) -> p k n", k=8)[:, 0:kn, 0:n], [Bp], [BPT])
                    else:
                        for k in range(kn):
                            evac(PT[0:kws[k], k, 0:n], pv[0:kws[k], k * 128:k * 128 + n], [Bp], [BPT])
                    for k in range(kn):
                        kt = k0 + k
                        S.op("pe", lambda k=k, kt=kt, PT=PT: nc.tensor.matmul(po[0:n, 0:132], lhsT=PT[0:kws[k], k, 0:n], rhs=V_[0:kws[k], kt, 0:132],
                             start=(kt == 0), stop=(kt == nkt - 1)), [BPT, BV_], [Bpo])
                S.op("dve", lambda: nc.vector.reciprocal(out=rv[0:n, 0:1], in_=po[0:n, 128:129]), [Bpo], [Brv])
                S.op("dve", lambda: nc.vector.tensor_scalar(out=ob[0:n, :], in0=po[0:n, 0:128], scalar1=rv[0:n, 0:1], scalar2=None, op0=ALU.mult), [Bpo, Brv], [Bob])
                S.dma("sp", OA[orow0:orow0 + n, h * 128:(h + 1) * 128], ob[0:n, :], reads=[Bob], writes=[S.buf("OA", orow0 // 128)])

            for h in range(8):
                for t in range(NT):
                    tq, Bt = tqs[t % 3]
                    S.dma("pool", tq[:], H[t * 128:(t + 1) * 128, C_QA + h * 128:C_QA + (h + 1) * 128], reads=[S.buf("H", t)], writes=[Bt])
                    transpose_into(qT[:, t * 128:(t + 1) * 128], tq[:], 128, 128, Bt, BqT)
                for t in range(NTP):
                    tq, Bt = tqs[t % 3]
                    S.dma("pool", tq[:], H[t * 128:(t + 1) * 128, C_KA + h * 128:C_KA + (h + 1) * 128], reads=[S.buf("H", t)], writes=[Bt])
                    transpose_into(kT[:, t * 128:(t + 1) * 128], tq[:], 128, 128, Bt, BkT)
                for t0 in range(0, NTP, 8):
                    t1 = min(NTP, t0 + 8)
                    S.dma("pool", V[:, t0:t1, 0:128], H[t0 * 128:t1 * 128, C_VA + h * 128:C_VA + (h + 1) * 128].rearrange("(t p) d -> p t d", p=128),
                          reads=[S.buf("H", t) for t in range(t0, t1)], writes=[BV])
                for t in range(NTP):
                    nk = 128 * (t + 1)
                    if t == 0:
                        attend(128, qT[:, 0:128], kT, BkT, V, BV, nk, 0, 0, NB[:, h, 128:256], 0, h)
                    else:
                        attend(128, qT[:, t * 128:(t + 1) * 128], kT, BkT, V, BV, nk, t * 128, nk - 256, NB[:, h, :], t * 128, h)
                for s in range(2):
                    for t in range(16):
                        tq, Bt = tqs[t % 3]
                        S.dma("pool", tq[:], cache_k[l, s, t * 128:(t + 1) * 128, h * 128:(h + 1) * 128], writes=[Bt])
                        transpose_into(kTs[:, t * 128:(t + 1) * 128], tq[:], 128, 128, Bt, BkTs)
                    r0 = NPK + 64 * s
                    tq, Bt = tqs[0]
                    S.dma("pool", tq[0:64, :], H[r0:r0 + 64, C_KA + h * 128:C_KA + (h + 1) * 128], reads=[S.buf("H", 33)], writes=[Bt])
                    transpose_into(kTs[:, 2048:2112], tq[0:64, :], 64, 128, Bt, BkTs)
                    for t0 in (0, 8):
                        S.dma("pool", Vs[:, t0:t0 + 8, 0:128], cache_v[l, s, t0 * 128:(t0 + 8) * 128, h * 128:(h + 1) * 128].rearrange("(t p) d -> p t d", p=128), writes=[BVs])
                    S.dma("pool", Vs[0:64, 16, 0:128], H[r0:r0 + 64, C_VA + h * 128:C_VA + (h + 1) * 128], reads=[S.buf("H", 33)], writes=[BVs])
                    attend(64, qT[:, r0:r0 + 64], kTs, BkTs, Vs, BVs, 2112, r0, 1920, NB[0:64, h, 0:192], r0, h)
            S.barrier()

    def phase_pool(l):
        with ExitStack() as es:
            bands = SB(es, "bands", [128, 3, 512], BF16); Bb = S.buf("bands")
            sprv = SB(es, "sprv", [16, 256], BF16)
            pw = SB(es, "pw", [128, 8, 256], BF16)
            psc = SB(es, "psc", [128, 1024])
            us = [(SB(es, "u", [128, 1024], BF16), S.buf("u", i)) for i in range(3)]
            dTs = [(SB(es, "dT", [128, 8, 128], BF16), S.buf("dT", i)) for i in range(2)]
            obs = [(SB(es, "pob", [128, 1024], BF16), S.buf("pob", i)) for i in range(2)]
            S.dma("pool", bands[:, 0, :], IN("c_pcur")[:, :], writes=[Bb])
            S.dma("pool", bands[:, 1, :], IN("c_pprv")[:, :], writes=[Bb])
            S.dma("pool", bands[:, 2, :], IN("c_pcur0")[:, :], writes=[Bb])
            S.dma("pool", sprv[:], IN("c_psprv")[:, :], writes=[Bb])
            S.dma("pool", pw[:], IN("pool_w")[l].rearrange("g (cc p) e -> p (g cc) e", p=128), writes=[Bb])
            S.dma("sp", psc[:], IN("pool_scale")[l:l + 1, :].to_broadcast([128, 1024]), writes=[Bb])
            cache_pool = IN("cache_pool")
            ui = [0]

            def unit(row0, n, u, Bu, up, Bup, npv, cur_i, prv_ap_fn):
                i = ui[0]; ui[0] += 1
                dT, BdT = dTs[i % 2]; ob, Bob = obs[i % 2]
                for g in range(4):
                    for cc in range(2):
                        c0 = g * 256 + cc * 128
                        p_, Bp = next_ps()
                        S.op("pe", lambda p_=p_, c0=c0, g=g: nc.tensor.matmul(p_[:, 0:n], lhsT=u[0:n, c0:c0 + 128], rhs=bands[0:n, cur_i, g * 128:g * 128 + n],
                             start=True, stop=(up is None)), [Bu, Bb], [Bp])
                        if up is not None:
                            S.op("pe", lambda p_=p_, c0=c0, g=g: nc.tensor.matmul(p_[:, 0:n], lhsT=up[0:npv, c0:c0 + 128], rhs=prv_ap_fn(g),
                                 start=False, stop=True), [Bup, Bb], [Bp])
                        evac(dT[:, g * 2 + cc, 0:n], p_[:, 0:n], [Bp], [BdT])
                for half in range(2):
                    p_, Bp = next_ps()
                    for gg in range(2):
                        g = half * 2 + gg
                        for cc in range(2):
                            S.op("pe", lambda p_=p_, g=g, gg=gg, cc=cc: nc.tensor.matmul(p_[0:n, gg * 256:(gg + 1) * 256], lhsT=dT[:, g * 2 + cc, 0:n],
                                 rhs=pw[:, g * 2 + cc, :], start=(cc == 0), stop=(cc == 1)), [BdT, Bb], [Bp])
                    S.op("dve", lambda p_=p_, half=half: nc.vector.tensor_tensor(out=ob[0:n, half * 512:(half + 1) * 512], in0=p_[0:n, :],
                         in1=psc[0:n, half * 512:(half + 1) * 512], op=ALU.mult), [Bp, Bb], [Bob])
                S.dma("sp", OB[row0:row0 + n, :], ob[0:n, :], reads=[Bob], writes=[S.buf("OB", row0 // 128)])

            prev = None
            for t in range(NTP):
                u, Bu = us[t % 3]
                S.dma("pool", u[:], H[t * 128:(t + 1) * 128, C_UB:C_UB + 1024], reads=[S.buf("H", t)], writes=[Bu])
                if t == 0:
                    unit(0, 128, u, Bu, None, None, 0, 2, None)
                else:
                    unit(t * 128, 128, u, Bu, prev[0], prev[1], 128, 0, lambda g: bands[:, 1, g * 128:(g + 1) * 128])
                prev = (u, Bu)
            for s in range(2):
                r0 = NPK + 64 * s
                u, Bu = us[(2 * s) % 3]; up, Bup = us[(2 * s + 1) % 3]
                S.dma("pool", u[0:64, :], H[r0:r0 + 64, C_UB:C_UB + 1024], reads=[S.buf("H", 33)], writes=[Bu])
                S.dma("pool", up[0:15, :], cache_pool[l, s, :, :], writes=[Bup])
                unit(r0, 64, u, Bu, up, Bup, 15, 0, lambda g: sprv[0:15, g * 64:(g + 1) * 64])
            S.barrier()

    def phase_ret(l):
        with ExitStack() as es:
            Sst = SB(es, "Sst", [128, 8, 128]); BS = S.buf("Sst")
            Sbf = SB(es, "Sbf", [128, 8, 128], BF16); BSb = S.buf("Sbf")
            tri = SB(es, "tri", [128, 128]); Btri = S.buf("tri")
            S.dma("sp", tri[:], IN("c_tri")[:, :], writes=[Btri])
            qcs = [(SB(es, "qc", [128, 1024]), S.buf("qc", i)) for i in range(2)]
            kcs = [(SB(es, "kc", [128, 1024]), S.buf("kc", i)) for i in range(2)]
            gcs = [(SB(es, "gc", [128, 1024]), S.buf("gc", i)) for i in range(2)]
            vbs = [(SB(es, "vb", [128, 1024], BF16), S.buf("vb", i)) for i in range(2)]
            css = [(SB(es, "cs", [128, 144]), S.buf("cs", i)) for i in range(2)]
            rq = SB(es, "rq", [128, 1024]); Brq = S.buf("rq")
            tmp = SB(es, "rtmp", [128, 512]); Btmp = S.buf("rtmp")
            qb = SB(es, "qb", [128, 1024], BF16); Bqb = S.buf("qb")
            kb_ = SB(es, "kb", [128, 1024], BF16); Bkb = S.buf("kb")
            qTt = SB(es, "qTt", [128, 8, 128], BF16); BqTt = S.buf("qTt")
            kTt = SB(es, "kTt", [128, 8, 128], BF16); BkTt = S.buf("kTt")
            WTs = [(SB(es, "WT", [128, 128], BF16), S.buf("WT", i)) for i in range(3)]
            osb = SB(es, "osb", [128, 1024]); Bosb = S.buf("osb")
            stt = SB(es, "rstt", [128, 8, 6]); mvv = SB(es, "rmv", [128, 8, 4]); Bst = S.buf("rst")
            ocb = [(SB(es, "ocb", [128, 1024], BF16), S.buf("ocb", i)) for i in range(2)]
            c_cos, c_sin, c_qdec, c_kdec = IN("c_cos"), IN("c_sin"), IN("c_qdec"), IN("c_kdec")
            state_ret = IN("state_ret")

            def rotary(src, n, cs, dst, Bsrc, Bcs, Bdst):
                s4 = src[0:n, :].rearrange("p (h two d) -> p h two d", h=8, two=2)
                d4 = dst[0:n, :].rearrange("p (h two d) -> p h two d", h=8, two=2)
                cosb = cs[0:n, 0:64].unsqueeze(1).to_broadcast([n, 8, 64])
                sinb = cs[0:n, 64:128].unsqueeze(1).to_broadcast([n, 8, 64])
                t3 = tmp[0:n, :].rearrange("p (h d) -> p h d", h=8)
                S.op("dve", lambda: nc.vector.tensor_tensor(out=d4[:, :, 0, :], in0=s4[:, :, 0, :], in1=cosb, op=ALU.mult), [Bsrc, Bcs], [Bdst])
                S.op("dve", lambda: nc.vector.tensor_tensor(out=t3, in0=s4[:, :, 1, :], in1=sinb, op=ALU.mult), [Bsrc, Bcs], [Btmp])
                S.op("dve", lambda: nc.vector.tensor_tensor(out=d4[:, :, 0, :], in0=d4[:, :, 0, :], in1=t3, op=ALU.subtract), [Bdst, Btmp], [Bdst])
                S.op("dve", lambda: nc.vector.tensor_tensor(out=d4[:, :, 1, :], in0=s4[:, :, 0, :], in1=sinb, op=ALU.mult), [Bsrc, Bcs], [Bdst])
                S.op("dve", lambda: nc.vector.tensor_tensor(out=t3, in0=s4[:, :, 1, :], in1=cosb, op=ALU.mult), [Bsrc, Bcs], [Btmp])
                S.op("dve", lambda: nc.vector.tensor_tensor(out=d4[:, :, 1, :], in0=d4[:, :, 1, :], in1=t3, op=ALU.add), [Bdst, Btmp], [Bdst])

            ui = [0]

            def unit(row0, n, gpow):
                i = ui[0]; ui[0] += 1
                qc, Bqc = qcs[i % 2]; kc, Bkc = kcs[i % 2]; gc, Bgc = gcs[i % 2]; vb, Bvb = vbs[i % 2]; cs, Bcs = css[i % 2]
                oc, Boc = ocb[i % 2]
                tb = S.buf("H", row0 // 128)
                S.dma("sp", qc[0:n, :], H[row0:row0 + n, C_QC:C_QC + 1024], reads=[tb], writes=[Bqc])
                S.dma("sp", kc[0:n, :], H[row0:row0 + n, C_KC:C_KC + 1024], reads=[tb], writes=[Bkc])
                S.dma("sp", gc[0:n, :], H[row0:row0 + n, C_GC:C_GC + 1024], reads=[tb], writes=[Bgc])
                S.dma("pool", vb[0:n, :], H[row0:row0 + n, C_VC:C_VC + 1024], reads=[tb], writes=[Bvb])
                S.dma("sp", cs[0:n, 0:64], c_cos[row0:row0 + n, :], writes=[Bcs])
                S.dma("sp", cs[0:n, 64:128], c_sin[row0:row0 + n, :], writes=[Bcs])
                S.dma("sp", cs[0:n, 128:136], c_qdec[row0:row0 + n, :], writes=[Bcs])
                S.dma("sp", cs[0:n, 136:144], c_kdec[row0:row0 + n, :], writes=[Bcs])
                rotary(qc, n, cs, rq, Bqc, Bcs, Brq)
                S.op("dve", lambda: nc.vector.tensor_tensor(out=qb[0:n, :].rearrange("p (h d) -> p h d", h=8), in0=rq[0:n, :].rearrange("p (h d) -> p h d", h=8),
                     in1=cs[0:n, 128:136].unsqueeze(2).to_broadcast([n, 8, 128]), op=ALU.mult), [Brq, Bcs], [Bqb])
                rotary(kc, n, cs, rq, Bkc, Bcs, Brq)
                S.op("dve", lambda: nc.vector.tensor_tensor(out=kb_[0:n, :].rearrange("p (h d) -> p h d", h=8), in0=rq[0:n, :].rearrange("p (h d) -> p h d", h=8),
                     in1=cs[0:n, 136:144].unsqueeze(2).to_broadcast([n, 8, 128]), op=ALU.mult), [Brq, Bcs], [Bkb])
                for src, Bsrc, dstT, BdstT in ((qb, Bqb, qTt, BqTt), (kb_, Bkb, kTt, BkTt)):
                    for h0 in range(0, 8, 4):
                        p_, Bp = next_ps()
                        pv = p_[:].bitcast(BF16)
                        for k in range(4):
                            S.op("pe", lambda k=k, pv=pv, src=src: nc.tensor.transpose(out=pv[:, k * 128:k * 128 + n], in_=src[0:n, (h0 + k) * 128:(h0 + k + 1) * 128],
                                 identity=ident_b[0:n, 0:n]), [Bsrc, Bc], [Bp])
                        evac(dstT[:, h0:h0 + 4, 0:n], pv[:, 0:512].rearrange("p (k n) -> p k n", k=4)[:, :, 0:n], [Bp], [BdstT])
                for hh in range(2):
                    po, Bpo = next_ps()
                    for k in range(4):
                        h = hh * 4 + k
                        p_, Bp = next_ps()
                        S.op("pe", lambda p_=p_, h=h: nc.tensor.matmul(p_[0:n, 0:n], lhsT=kTt[:, h, 0:n], rhs=qTt[:, h, 0:n], start=True, stop=True), [BkTt, BqTt], [Bp])
                        WT, BWT = WTs[h % 3]
                        S.op("dve", lambda p_=p_, WT=WT: nc.vector.tensor_tensor(out=WT[0:n, 0:n], in0=p_[0:n, 0:n], in1=tri[0:n, 0:n], op=ALU.mult), [Bp, Btri], [BWT])
                        S.op("pe", lambda h=h, k=k, WT=WT, po=po: nc.tensor.matmul(po[0:n, k * 128:(k + 1) * 128], lhsT=WT[0:n, 0:n], rhs=vb[0:n, h * 128:(h + 1) * 128],
                             start=True, stop=False), [BWT, Bvb], [Bpo])
                        S.op("pe", lambda h=h, k=k, po=po: nc.tensor.matmul(po[0:n, k * 128:(k + 1) * 128], lhsT=qTt[:, h, 0:n], rhs=Sbf[:, h, :],
                             start=False, stop=True), [BqTt, BSb], [Bpo])
                    evac(osb[0:n, hh * 512:(hh + 1) * 512], po[0:n, :], [Bpo], [Bosb])
                for h in range(8):
                    p_, Bp = next_ps()
                    S.op("pe", lambda p_=p_, h=h: nc.tensor.matmul(p_[:, 0:128], lhsT=kb_[0:n, h * 128:(h + 1) * 128], rhs=vb[0:n, h * 128:(h + 1) * 128],
                         start=True, stop=True), [Bkb, Bvb], [Bp])
                    g = float(GAM[h] ** gpow)
                    S.op("dve", lambda h=h, g=g: nc.vector.tensor_scalar(out=Sst[:, h, :], in0=Sst[:, h, :], scalar1=g, scalar2=None, op0=ALU.mult), [BS], [BS])
                    S.op("dve", lambda h=h, g=g, p_=p_: nc.vector.scalar_tensor_tensor(out=Sst[:, h, :], in0=p_[:, 0:128], scalar=g, in1=Sst[:, h, :],
                         op0=ALU.mult, op1=ALU.add), [Bp, BS], [BS])
                S.op("act", lambda: nc.scalar.activation(out=Sbf[:], in_=Sst[:], func=AF.Copy), [BS], [BSb])
                for h in range(8):
                    S.op("dve", lambda h=h: nc.vector.bn_stats(out=stt[0:n, h, :], in_=osb[0:n, h * 128:(h + 1) * 128]), [Bosb], [Bst])
                    S.op("dve", lambda h=h: nc.vector.bn_aggr(out=mvv[0:n, h, 0:2], in_=stt[0:n, h, :]), [Bst], [Bst])
                rstd_from_var(mvv[0:n, :, 1:2], mvv[0:n, :, 3:4], mvv[0:n, :, 2:3], Bst)
                for h in range(8):
                    S.op("dve", lambda h=h: nc.vector.tensor_scalar(out=osb[0:n, h * 128:(h + 1) * 128], in0=osb[0:n, h * 128:(h + 1) * 128],
                         scalar1=mvv[0:n, h, 0:1], scalar2=mvv[0:n, h, 2:3], op0=ALU.subtract, op1=ALU.mult), [Bosb, Bst], [Bosb])
                S.op("act", lambda: nc.scalar.activation(out=gc[0:n, :], in_=gc[0:n, :], func=AF.Silu), [Bgc], [Bgc])
                S.op("dve", lambda: nc.vector.tensor_tensor(out=oc[0:n, :], in0=osb[0:n, :], in1=gc[0:n, :], op=ALU.mult), [Bosb, Bgc], [Boc])
                S.dma("sp", OC[row0:row0 + n, :], oc[0:n, :], reads=[Boc], writes=[S.buf("OC", row0 // 128)])

            S.op("dve", lambda: nc.vector.memset(Sst[:], 0.0), [], [BS])
            S.op("dve", lambda: nc.vector.memset(Sbf[:], 0.0), [], [BSb])
            for t in range(NTP):
                unit(t * 128, 128, 128 if t < 32 else 64)
            S.dma("sp", o_retp[l].rearrange("h k v -> k h v"), Sst[:], reads=[BS], writes=[S.buf("out")])
            for s in range(2):
                S.dma("sp", Sst[:], state_ret[l, s].rearrange("h k v -> k h v"), writes=[BS])
                S.op("act", lambda: nc.scalar.activation(out=Sbf[:], in_=Sst[:], func=AF.Copy), [BS], [BSb])
                unit(NPK + 64 * s, 64, 64)
                S.dma("sp", o_rets[l, s].rearrange("h k v -> k h v"), Sst[:], reads=[BS], writes=[S.buf("out")])
            S.barrier()

    def phase_merge(l):
        w_gate, b_gate, w_branch, w_out = IN("w_gate"), IN("b_gate"), IN("w_branch"), IN("w_out")
        NS = 6
        with ExitStack() as es:
            xT = SB(es, "cxT", [128, 40, NS * 128], BF16); BxT = S.buf("cxT")
            tmpl = [(SB(es, "ctA", [128, D], BF16), S.buf("ctA", i)) for i in range(2)]
            wgs = [(SB(es, "cwg", [128, 16, 512], BF16), S.buf("cwg", i)) for i in range(2)]
            wbs = [(SB(es, "cwb", [128, 8, 512], BF16), S.buf("cwb", i)) for i in range(2)]
            bgb = SB(es, "bgb", [1, 3 * D], BF16); Bbg = S.buf("bgb")
            zacc = SB(es, "zacc", [128, NS, 512]); Bz = S.buf("zacc")
            sgs = [(SB(es, "sg", [128, 512]), S.buf("sg", i)) for i in range(2)]
            zbs = [(SB(es, "zb", [128, 512], BF16), S.buf("zb", i)) for i in range(2)]
            S.dma("pool", bgb[:], b_gate[l:l + 1, :], writes=[Bbg])
            wi = 0; si = 0
            for tiles in chunks(NT, NS):
                load_T(xT, 0, BxT, X, 0, D, tiles, "X", tmpl)
                load_T(xT, 16, BxT, OA, 0, 1024, tiles, "OA", tmpl)
                load_T(xT, 24, BxT, OB, 0, 1024, tiles, "OB", tmpl)
                load_T(xT, 32, BxT, OC, 0, 1024, tiles, "OC", tmpl)
                for cb in range(4):
                    for i in range(3):
                        wg, Bwg = wgs[wi % 2]; wb, Bwb = wbs[wi % 2]; wi += 1
                        gc0 = i * D + cb * 512
                        S.dma("pool", wg[:], w_gate[l, :, gc0:gc0 + 512].rearrange("(k p) n -> p k n", p=128), writes=[Bwg])
                        S.dma("pool", wb[:], w_branch[l, i, :, cb * 512:(cb + 1) * 512].rearrange("(k p) n -> p k n", p=128), writes=[Bwb])
                        for j, t in enumerate(tiles):
                            pg, Bpg = next_ps()
                            for k in range(16):
                                S.op("pe", lambda k=k, pg=pg, wg=wg, j=j: nc.tensor.matmul(pg[:, :], lhsT=xT[:, k, j * 128:(j + 1) * 128], rhs=wg[:, k, :],
                                     start=(k == 0), stop=False), [BxT, Bwg], [Bpg])
                            S.op("pe", lambda pg=pg: nc.tensor.matmul(pg[:, :], lhsT=ones_b[0:1, :], rhs=bgb[0:1, gc0:gc0 + 512], start=False, stop=True), [Bc, Bbg], [Bpg])
                            pu, Bpu = next_ps()
                            for k in range(8):
                                S.op("pe", lambda k=k, pu=pu, wb=wb, j=j: nc.tensor.matmul(pu[:, :], lhsT=xT[:, 16 + i * 8 + k, j * 128:(j + 1) * 128], rhs=wb[:, k, :],
                                     start=(k == 0), stop=(k == 7)), [BxT, Bwb], [Bpu])
                            sg, Bsg = sgs[si % 2]; si += 1
                            S.op("act", lambda pg=pg, sg=sg: nc.scalar.activation(out=sg[:], in_=pg[:, :], func=AF.Sigmoid), [Bpg], [Bsg])
                            if i == 0:
                                S.op("dve", lambda sg=sg, pu=pu, j=j: nc.vector.tensor_tensor(out=zacc[:, j, :], in0=sg[:], in1=pu[:, :], op=ALU.mult), [Bsg, Bpu], [Bz])
                            else:
                                S.op("dve", lambda sg=sg, pu=pu: nc.vector.tensor_tensor(out=sg[:], in0=sg[:], in1=pu[:, :], op=ALU.mult), [Bsg, Bpu], [Bsg])
                                S.op("dve", lambda sg=sg, j=j: nc.vector.tensor_tensor(out=zacc[:, j, :], in0=zacc[:, j, :], in1=sg[:], op=ALU.add), [Bsg, Bz], [Bz])
                    for j, t in enumerate(tiles):
                        zb, Bzb = zbs[j % 2]
                        S.op("act", lambda zb=zb, j=j: nc.scalar.activation(out=zb[:], in_=zacc[:, j, :], func=AF.Copy), [Bz], [Bzb])
                        S.dma("sp", Z[t * 128:(t + 1) * 128, cb * 512:(cb + 1) * 512], zb[:], reads=[Bzb], writes=[S.buf("Z", t)])
            S.barrier()
        with ExitStack() as es:
            zT = SB(es, "zT", [128, 16, NS * 128], BF16); BzT = S.buf("zT")
            tmpl = [(SB(es, "dtA", [128, D], BF16), S.buf("dtA", i)) for i in range(2)]
            wos = [(SB(es, "wo", [128, 16, 512], BF16), S.buf("wo", i)) for i in range(2)]
            rowb = SB(es, "rowb", [128, NS, D]); Brow = S.buf("rowb")
            g_bc = SB(es, "g1", [128, D]); b_bc = SB(es, "b1", [128, D])
            xts = [(SB(es, "x1t", [128, D]), S.buf("x1t", i)) for i in range(2)]
            st = SB(es, "st1", [128, 24]); mv = SB(es, "mv1", [128, 4]); Bst = S.buf("st1")
            S.dma("sp", g_bc[:], IN("ln1_g")[l:l + 1, :].to_broadcast([128, D]), writes=[Bc])
            S.dma("sp", b_bc[:], IN("ln1_b")[l:l + 1, :].to_broadcast([128, D]), writes=[Bc])
            wi = 0
            for tiles in chunks(NT, NS):
                load_T(zT, 0, BzT, Z, 0, D, tiles, "Z", tmpl)
                for cb in range(4):
                    wo, Bwo = wos[wi % 2]; wi += 1
                    S.dma("pool", wo[:], w_out[l, :, cb * 512:(cb + 1) * 512].rearrange("(k p) n -> p k n", p=128), writes=[Bwo])
                    for j, t in enumerate(tiles):
                        p_, Bp = next_ps()
                        for k in range(16):
                            S.op("pe", lambda k=k, p_=p_, wo=wo, j=j: nc.tensor.matmul(p_[:, :], lhsT=zT[:, k, j * 128:(j + 1) * 128], rhs=wo[:, k, :],
                                 start=(k == 0), stop=(k == 15)), [BzT, Bwo], [Bp])
                        evac(rowb[:, j, cb * 512:(cb + 1) * 512], p_[:, :], [Bp], [Brow])
                for j, t in enumerate(tiles):
                    xt, Bxt = xts[j % 2]
                    S.dma("sp", xt[:], X[t * 128:(t + 1) * 128, :], reads=[S.buf("X", t)], writes=[Bxt])
                    S.op("dve", lambda xt=xt, j=j: nc.vector.scalar_tensor_tensor(out=rowb[:, j, :], in0=xt[:], scalar=ALPHA, in1=rowb[:, j, :],
                         op0=ALU.mult, op1=ALU.add), [Bxt, Brow], [Brow])
                    ln_tile(rowb[:, j, :], xt[:], g_bc[:], b_bc[:], st[:], mv[:], Brow, Bxt, Bst, mask_col=mask_of(t))
                    S.dma("sp", X1[t * 128:(t + 1) * 128, :], xt[:], reads=[Bxt], writes=[S.buf("X1", t)])
            S.barrier()

    def phase_ffn(l):
        NS = 5
        moe = (l % 2 == 1)
        nexp = 8 if moe else 1
        with ExitStack() as es:
            x1T = SB(es, "x1T", [128, 16, NS * 128], BF16); Bx1T = S.buf("x1T")
            actT = SB(es, "actT", [128, 44, NS * 128], BF16); Bact = S.buf("actT")
            rowb = SB(es, "frow", [128, NS, D]); Brow = S.buf("frow")
            sgs = [(SB(es, "fsg", [128, 512]), S.buf("fsg", i)) for i in range(2)]
            gates = SB(es, "gates", [128, NS, 32]); Bga = S.buf("gates")
            if moe:
                wr = SB(es, "wr", [128, 16, 8]); brr = SB(es, "brr", [1, 8]); Bwr = S.buf("wr")
                S.dma("sp", wr[:], IN("moe_w_router").rearrange("(k p) e -> p k e", p=128), writes=[Bwr])
                S.dma("sp", brr[:], IN("moe_b_router")[:, :], writes=[Bwr])
            wi = 0; wdi = 0; si = 0
            for tiles in chunks(NT, NS):
                ntk = len(tiles) * 128
                with ExitStack() as es2:
                    tmpl = [(SB(es2, "etA", [128, D], BF16), S.buf("etA", 0))] * 2
                    load_T(x1T, 0, Bx1T, X1, 0, D, tiles, "X1", tmpl)
                    if moe:
                        xf = SB(es2, "xf", [128, D]); Bxf = S.buf("xf")
                        xfT = SB(es2, "xfT", [128, 16, 128]); BxfT = S.buf("xfT")
                        for j, t in enumerate(tiles):
                            S.dma("sp", xf[:], X1[t * 128:(t + 1) * 128, :], reads=[S.buf("X1", t)], writes=[Bxf])
                            for k0 in range(0, 16, 4):
                                p_, Bp = next_ps()
                                for k in range(4):
                                    S.op("pe", lambda k=k, p_=p_: nc.tensor.transpose(out=p_[:, k * 128:(k + 1) * 128], in_=xf[:, (k0 + k) * 128:(k0 + k + 1) * 128],
                                         identity=ident_f[:]), [Bxf, Bc], [Bp])
                                evac(xfT[:, k0:k0 + 4, :], p_[:, :].rearrange("p (k n) -> p k n", k=4), [Bp], [BxfT])
                            p_, Bp = next_ps()
                            for k in range(16):
                                S.op("pe", lambda k=k, p_=p_: nc.tensor.matmul(p_[:, 0:8], lhsT=xfT[:, k, :], rhs=wr[:, k, :], start=(k == 0), stop=False), [BxfT, Bwr], [Bp])
                            S.op("pe", lambda p_=p_: nc.tensor.matmul(p_[:, 0:8], lhsT=ones_f[0:1, :], rhs=brr[0:1, :], start=False, stop=True), [Bc, Bwr], [Bp])
                            G = gates[:, j, :]
                            S.op("dve", lambda p_=p_, G=G: nc.vector.tensor_copy(out=G[:, 0:8], in_=p_[:, 0:8]), [Bp], [Bga])
                            S.op("dve", lambda G=G: nc.vector.max(out=G[:, 8:16], in_=G[:, 0:8]), [Bga], [Bga])
                            S.op("dve", lambda G=G: nc.vector.tensor_tensor(out=G[:, 24:25], in0=G[:, 8:9], in1=G[:, 9:10], op=ALU.subtract), [Bga], [Bga])
                            S.op("act", lambda G=G: nc.scalar.activation(out=G[:, 25:26], in_=G[:, 24:25], func=AF.Sigmoid), [Bga], [Bga])
                            S.op("dve", lambda G=G: nc.vector.tensor_scalar(out=G[:, 26:27], in0=G[:, 25:26], scalar1=-1.0, scalar2=1.0, op0=ALU.mult, op1=ALU.add), [Bga], [Bga])
                            S.op("dve", lambda G=G: nc.vector.tensor_scalar(out=G[:, 16:24], in0=G[:, 0:8], scalar1=G[:, 8:9], scalar2=G[:, 25:26], op0=ALU.is_equal, op1=ALU.mult), [Bga], [Bga])
                            S.op("dve", lambda G=G: nc.vector.tensor_scalar(out=G[:, 0:8], in0=G[:, 0:8], scalar1=G[:, 9:10], scalar2=G[:, 26:27], op0=ALU.is_equal, op1=ALU.mult), [Bga], [Bga])
                            S.op("dve", lambda G=G: nc.vector.tensor_tensor(out=G[:, 16:24], in0=G[:, 16:24], in1=G[:, 0:8], op=ALU.add), [Bga], [Bga])
                S.barrier()
                es3 = ExitStack()
                wgs = [(SB(es3, "fwg", [128, 16, 256], BF16), S.buf("fwg", i)) for i in range(2)]
                wus = [(SB(es3, "fwu", [128, 16, 256], BF16), S.buf("fwu", i)) for i in range(2)]
                wds = [(SB(es3, "fwd", [128, 44, 256], BF16), S.buf("fwd", i)) for i in range(2)]
                for e in range(nexp):
                    if moe:
                        Wg, Wu, Wd = IN("moe_w_gate")[e], IN("moe_w_up")[e], IN("moe_w_down")[e]
                    else:
                        Wg, Wu, Wd = IN("ffn_w_gate"), IN("ffn_w_up"), IN("ffn_w_down")
                    for f0 in range(0, DFF, 256):
                        wg, Bwg = wgs[wi % 2]; wu, Bwu = wus[wi % 2]; wi += 1
                        S.dma("pool", wg[:], Wg[:, f0:f0 + 256].rearrange("(k p) n -> p k n", p=128), writes=[Bwg])
                        S.dma("pool", wu[:], Wu[:, f0:f0 + 256].rearrange("(k p) n -> p k n", p=128), writes=[Bwu])
                        for fc in range(2):
                            c = f0 // 128 + fc
                            for n0 in range(0, ntk, 512):
                                nw = min(512, ntk - n0)
                                pg, Bpg = next_ps(); pu, Bpu = next_ps()
                                for k in range(16):
                                    S.op("pe", lambda k=k, pg=pg, wg=wg: nc.tensor.matmul(pg[:, 0:nw], lhsT=wg[:, k, fc * 128:(fc + 1) * 128], rhs=x1T[:, k, n0:n0 + nw],
                                         start=(k == 0), stop=(k == 15)), [Bx1T, Bwg], [Bpg])
                                for k in range(16):
                                    S.op("pe", lambda k=k, pu=pu, wu=wu: nc.tensor.matmul(pu[:, 0:nw], lhsT=wu[:, k, fc * 128:(fc + 1) * 128], rhs=x1T[:, k, n0:n0 + nw],
                                         start=(k == 0), stop=(k == 15)), [Bx1T, Bwu], [Bpu])
                                sg, Bsg = sgs[si % 2]; si += 1
                                S.op("act", lambda pg=pg, sg=sg: nc.scalar.activation(out=sg[:, 0:nw], in_=pg[:, 0:nw], func=AF.Silu), [Bpg], [Bsg])
                                S.op("dve", lambda pu=pu, sg=sg, c=c: nc.vector.tensor_tensor(out=actT[:, c, n0:n0 + nw], in0=sg[:, 0:nw], in1=pu[:, 0:nw], op=ALU.mult),
                                     [Bsg, Bpu], [Bact])
                    for c0 in range(0, D, 256):
                        wd, Bwd = wds[wdi % 2]; wdi += 1
                        S.dma("pool", wd[:], Wd[:, c0:c0 + 256].rearrange("(k p) n -> p k n", p=128), writes=[Bwd])
                        for j0 in range(0, len(tiles), 2):
                            jn = min(2, len(tiles) - j0)
                            p_, Bp = next_ps()
                            for jj in range(jn):
                                j = j0 + jj
                                for k in range(44):
                                    S.op("pe", lambda k=k, p_=p_, wd=wd, j=j, jj=jj: nc.tensor.matmul(p_[:, jj * 256:(jj + 1) * 256], lhsT=actT[:, k, j * 128:(j + 1) * 128], rhs=wd[:, k, :],
                                         start=(k == 0), stop=(k == 43)), [Bact, Bwd], [Bp])
                            for jj in range(jn):
                                j = j0 + jj
                                if not moe:
                                    evac(rowb[:, j, c0:c0 + 256], p_[:, jj * 256:(jj + 1) * 256], [Bp], [Brow])
                                elif e == 0:
                                    S.op("dve", lambda p_=p_, j=j, jj=jj: nc.vector.tensor_scalar(out=rowb[:, j, c0:c0 + 256], in0=p_[:, jj * 256:(jj + 1) * 256],
                                         scalar1=gates[:, j, 16 + e:17 + e], scalar2=None, op0=ALU.mult), [Bp, Bga], [Brow])
                                else:
                                    S.op("dve", lambda p_=p_, j=j, jj=jj, e=e: nc.vector.scalar_tensor_tensor(out=rowb[:, j, c0:c0 + 256], in0=p_[:, jj * 256:(jj + 1) * 256],
                                         scalar=gates[:, j, 16 + e:17 + e], in1=rowb[:, j, c0:c0 + 256], op0=ALU.mult, op1=ALU.add), [Bp, Bga, Brow], [Brow])
                S.barrier()
                es3.close()
                with ExitStack() as es2:
                    g_bc = SB(es2, "g2", [128, D]); b_bc = SB(es2, "b2", [128, D])
                    xts = [(SB(es2, "x2t", [128, D]), S.buf("x2t", 0))] * 2
                    st = SB(es2, "st2", [128, 24]); mv = SB(es2, "mv2", [128, 4]); Bst = S.buf("st2")
                    S.dma("sp", g_bc[:], IN("ln2_g")[l:l + 1, :].to_broadcast([128, D]), writes=[Bc])
                    S.dma("sp", b_bc[:], IN("ln2_b")[l:l + 1, :].to_broadcast([128, D]), writes=[Bc])
                    for j, t in enumerate(tiles):
                        xt, Bxt = xts[j % 2]
                        S.dma("sp", xt[:], X1[t * 128:(t + 1) * 128, :], reads=[S.buf("X1", t)], writes=[Bxt])
                        S.op("dve", lambda xt=xt, j=j: nc.vector.scalar_tensor_tensor(out=rowb[:, j, :], in0=xt[:], scalar=ALPHA, in1=rowb[:, j, :],
                             op0=ALU.mult, op1=ALU.add), [Bxt, Brow], [Brow])
                        ln_tile(rowb[:, j, :], xt[:], g_bc[:], b_bc[:], st[:], mv[:], Brow, Bxt, Bst, mask_col=mask_of(t))
                        S.dma("sp", X[t * 128:(t + 1) * 128, :], xt[:], reads=[Bxt], writes=[S.buf("X", t)])
                        if l == 1:
                            S.dma("sp", o_y[t * 128:(t + 1) * 128, :], xt[:], reads=[Bxt], writes=[S.buf("out")])
                    S.barrier()
            S.barrier()

    for l in range(2):
        if on("A"):
            phase_A(l)
        if on("index"):
            phase_index(l)
        if on("attn"):
            phase_attn(l)
        if on("pool"):
            phase_pool(l)
        if on("ret"):
            phase_ret(l)
        if on("merge"):
            phase_merge(l)
        if on("ffn"):
            phase_ffn(l)
        if STOP_AFTER is not None and STOP_AFTER[1] == l:
            break

    S.finish()
    return nc, P


_CACHE = {}


def kernel(**inp):
    f32 = np.float32
    inp = {k: np.asarray(v) for k, v in inp.items()}
    if "nc" not in _CACHE:
        _CACHE["nc"] = build_program()
    nc, P = _CACHE["nc"]
    consts = host_consts()
    xp, xs = inp["x_prompt"], inp["x_sample"]
    in_maps = []
    for c in range(8):
        b = c % 2
        xin = np.zeros((NTOK, D), f32)
        xin[FRONT:FRONT + 16] = inp["meta_tokens"]
        xin[64:64 + 4096] = xp[b]
        xin[NPK:NPK + 64] = xs[2 * c]
        xin[NPK + 64:] = xs[2 * c + 1]
        m = {
            "xin": xin,
            "cache_k": np.ascontiguousarray(inp["cache_k"][:, 2 * c:2 * c + 2].reshape(2, 2, 2048, 1024)),
            "cache_v": np.ascontiguousarray(inp["cache_v"][:, 2 * c:2 * c + 2].reshape(2, 2, 2048, 1024)),
            "cache_ki": np.ascontiguousarray(inp["cache_ki"][:, 2 * c:2 * c + 2]),
            "cache_pool": np.ascontiguousarray(inp["cache_pool"][:, 2 * c:2 * c + 2]),
            "state_ret": np.ascontiguousarray(inp["state_ret"][:, 2 * c:2 * c + 2]),
            "ln_in_g": inp["ln_in_g"].reshape(1, D), "ln_in_b": inp["ln_in_b"].reshape(1, D),
            "w_in": inp["w_in"], "t5_bias": inp["t5_bias"].reshape(1, 256),
            "pool_w": inp["pool_w"], "pool_scale": inp["pool_scale"], "w_branch": inp["w_branch"],
            "w_gate": inp["w_gate"], "b_gate": inp["b_gate"], "w_out": inp["w_out"],
            "ln1_g": inp["ln1_g"], "ln1_b": inp["ln1_b"], "ln2_g": inp["ln2_g"], "ln2_b": inp["ln2_b"],
            "ffn_w_gate": inp["ffn_w_gate"][0], "ffn_w_up": inp["ffn_w_up"][0], "ffn_w_down": inp["ffn_w_down"][0],
            "moe_w_router": inp["moe_w_router"][0], "moe_b_router": inp["moe_b_router"].reshape(1, 8),
            "moe_w_gate": inp["moe_w_gate"][0], "moe_w_up": inp["moe_w_up"][0], "moe_w_down": inp["moe_w_down"][0],
        }
        m.update(consts)
        in_maps.append({k: np.ascontiguousarray(v, dtype=f32) for k, v in m.items() if k in USED_INPUTS})
    if DEV_CORES is not None:
        res = run_bass_kernel_spmd(nc, [in_maps[c] for c in DEV_CORES], core_ids=list(range(len(DEV_CORES))))
        return res.results
    res = run_bass_kernel_spmd(nc, in_maps, core_ids=list(range(8)))
    R = res.results
    T = 4112
    y_prompt = np.stack([R[b]["o_y"][64:64 + 4096] for b in range(2)])
    y_sample = np.concatenate([R[c]["o_y"][NPK:].reshape(2, 64, D) for c in range(8)])

    def pr(name, w):
        return np.stack([np.stack([R[b][name][l][FRONT:FRONT + T] for b in range(2)]) for l in range(2)])

    def sm(name, w):
        return np.stack([np.concatenate([R[c][name][l][NPK:].reshape(2, 64, w) for c in range(8)]) for l in range(2)])

    k_p = pr("o_k", 1024).reshape(2, 2, T, 8, 128); v_p = pr("o_v", 1024).reshape(2, 2, T, 8, 128); ki_p = pr("o_ki", 64)
    pool_p = np.stack([np.stack([R[b]["o_u"][l][FRONT + T - 15:FRONT + T] for b in range(2)]) for l in range(2)])
    ret_p = np.stack([np.stack([R[b]["o_retp"][l] for b in range(2)]) for l in range(2)])
    k_s = sm("o_k", 1024).reshape(2, 16, 64, 8, 128); v_s = sm("o_v", 1024).reshape(2, 16, 64, 8, 128); ki_s = sm("o_ki", 64)
    pool_s = sm("o_u", 1024)[:, :, 49:64]
    ret_s = np.stack([np.concatenate([R[c]["o_rets"][l] for c in range(8)]) for l in range(2)])
    return (y_prompt.astype(f32), y_sample.astype(f32), k_p, v_p, ki_p, pool_p, ret_p, k_s, v_s, ki_s, pool_s, ret_s)
```
